# Optimizing a Trainium2 kernel written in Bass

```python
import math
import jax
import jax.numpy as jnp
from jax import lax
import numpy as np

D_MODEL = 1024
BATCH = 2
SEQ = 16384
DEPTH = 4

N_MIXERS = 3
MOBA_HEADS = 8
MOBA_HEAD_DIM = D_MODEL // MOBA_HEADS
MOBA_BLOCK = 256
MOBA_TOPK = 3
MOBA_Q_BLOCK = 64
SCONV_WIDTH = 3
GDN_HEADS = 8
GDN_HEAD_DIM = D_MODEL // GDN_HEADS
GDN_CONV_WIDTH = 4
GDN_CHUNK = 64
FFN_DIM = 2816
FFN_CONV_WIDTH = 3

NORM_EPS = 1e-6
NEG_INF = -1e30

N_MOBA = (DEPTH + 2) // 3
N_SCONV = (DEPTH + 1) // 3
N_GDN = DEPTH // 3

kernel_name = 'hybrid_moba_shortconv_gdn_convffn'


def rmsnorm(x, g):
    xf = x.astype(jnp.float32)
    y = xf * lax.rsqrt(jnp.mean(xf * xf, axis=-1, keepdims=True) + NORM_EPS)
    return (y * g.astype(jnp.float32)).astype(x.dtype)


def l2norm(x):
    return x * lax.rsqrt(jnp.sum(x * x, axis=-1, keepdims=True) + NORM_EPS)


def causal_dwconv(x, w):
    width = w.shape[0]
    seq = x.shape[1]
    xp = jnp.pad(x, ((0, 0), (width - 1, 0), (0, 0)))
    out = w[width - 1] * x
    for k in range(width - 1):
        out = out + w[k] * xp[:, k:k + seq]
    return out


def alibi_slopes(n_heads):
    return jnp.exp2(-8.0 * jnp.arange(1, n_heads + 1, dtype=jnp.float32) / n_heads)


def moba_attention(h, w_qkv, w_o):
    bsz, seq, _ = h.shape
    nh, hd, blk, qb = MOBA_HEADS, MOBA_HEAD_DIM, MOBA_BLOCK, MOBA_Q_BLOCK
    qkv = (h @ w_qkv).reshape(bsz, seq, 3, nh, hd)
    q, k, v = [jnp.swapaxes(qkv[:, :, i], 1, 2) for i in range(3)]
    n_blk = -(-seq // blk)
    pad = n_blk * blk - seq
    k_blocks = jnp.pad(k, ((0, 0), (0, 0), (0, pad), (0, 0))).reshape(bsz, nh, n_blk, blk, hd)
    v_blocks = jnp.pad(v, ((0, 0), (0, 0), (0, pad), (0, 0))).reshape(bsz, nh, n_blk, blk, hd)
    k_mean = jnp.mean(k_blocks.astype(jnp.float32), axis=3)
    n_top = min(MOBA_TOPK, n_blk)
    slopes = alibi_slopes(nh)
    scale = hd ** -0.5
    n_qb = seq // qb
    q_chunks = jnp.moveaxis(q.reshape(bsz, nh, n_qb, qb, hd), 2, 0)
    b_idx = jnp.arange(bsz)[:, None, None, None]
    h_idx = jnp.arange(nh)[None, :, None, None]
    blk_ids = jnp.arange(n_blk)
    offs = jnp.arange(blk)

    def attend(args):
        c, qc = args
        pos_q = c * qb + jnp.arange(qb)
        own = (c * qb) // blk
        gate = jnp.einsum('bhqd,bhnd->bhqn', qc.astype(jnp.float32), k_mean)
        gate = jnp.where(blk_ids < own, gate, NEG_INF)
        _, sel = lax.top_k(gate, n_top)
        sel_ok = jnp.arange(n_top) < own
        k_sel = k_blocks[b_idx, h_idx, sel]
        v_sel = v_blocks[b_idx, h_idx, sel]
        k_own = lax.dynamic_index_in_dim(k_blocks, own, axis=2, keepdims=False)
        v_own = lax.dynamic_index_in_dim(v_blocks, own, axis=2, keepdims=False)
        s_sel = jnp.einsum('bhqd,bhqjkd->bhqjk', qc, k_sel).astype(jnp.float32) * scale
        dist_sel = (pos_q[:, None, None] - (sel[..., None] * blk + offs)).astype(jnp.float32)
        s_sel = s_sel - slopes[:, None, None, None] * dist_sel
        s_sel = jnp.where(sel_ok[:, None], s_sel, NEG_INF)
        dist_own = pos_q[:, None] - (own * blk + offs)[None, :]
        s_own = jnp.einsum('bhqd,bhkd->bhqk', qc, k_own).astype(jnp.float32) * scale
        s_own = s_own - slopes[:, None, None] * dist_own.astype(jnp.float32)
        s_own = jnp.where(dist_own >= 0, s_own, NEG_INF)
        scores = jnp.concatenate([s_sel.reshape(bsz, nh, qb, n_top * blk), s_own], axis=-1)
        p = jax.nn.softmax(scores, axis=-1).astype(v.dtype)
        p_sel = p[..., :n_top * blk].reshape(bsz, nh, qb, n_top, blk)
        p_own = p[..., n_top * blk:]
        return (jnp.einsum('bhqjk,bhqjkd->bhqd', p_sel, v_sel)
                + jnp.einsum('bhqk,bhkd->bhqd', p_own, v_own))

    o = lax.map(attend, (jnp.arange(n_qb), q_chunks))
    o = jnp.moveaxis(o, 0, 2).reshape(bsz, nh, seq, hd)
    o = jnp.swapaxes(o, 1, 2).reshape(bsz, seq, nh * hd)
    return o @ w_o


def short_conv_mixer(h, w_in, conv_w, w_out):
    d = D_MODEL
    proj = h @ w_in
    b_gate, c_gate, xv = proj[..., :d], proj[..., d:2 * d], proj[..., 2 * d:]
    return (b_gate * causal_dwconv(c_gate * xv, conv_w)) @ w_out


def gated_deltanet(h, w_in, conv_w, a_log, dt_bias, norm_w, w_o):
    bsz, seq, _ = h.shape
    nh, dh, cs = GDN_HEADS, GDN_HEAD_DIM, GDN_CHUNK
    d = nh * dh
    f32 = jnp.float32
    proj = h @ w_in
    qkv = jax.nn.silu(causal_dwconv(proj[..., :3 * d], conv_w))
    z = proj[..., 3 * d:4 * d].reshape(bsz, seq, nh, dh).astype(f32)
    b_logit = proj[..., 4 * d:4 * d + nh].astype(f32)
    a_in = proj[..., 4 * d + nh:].astype(f32)
    q, k, v = [qkv[..., i * d:(i + 1) * d].reshape(bsz, seq, nh, dh).astype(f32) for i in range(3)]
    q = l2norm(q) * (dh ** -0.5)
    k = l2norm(k)
    beta = jax.nn.sigmoid(b_logit)
    g = -jnp.exp(a_log.astype(f32)) * jax.nn.softplus(a_in + dt_bias.astype(f32))
    n_ch = seq // cs

    def chunks(t):
        t = jnp.moveaxis(t, 2, 1)
        return t.reshape((bsz, nh, n_ch, cs) + t.shape[3:])

    q, k, v, beta, g = chunks(q), chunks(k), chunks(v), chunks(beta), chunks(g)
    gc = jnp.cumsum(g, axis=-1)
    idx = jnp.arange(cs)
    incl = idx[:, None] >= idx[None, :]
    strict = idx[:, None] > idx[None, :]
    diff = gc[..., :, None] - gc[..., None, :]
    decay = jnp.where(incl, jnp.exp(jnp.where(incl, diff, 0.0)), 0.0)
    k_beta = k * beta[..., None]
    v_beta = v * beta[..., None]
    a_mat = jnp.where(strict, jnp.einsum('bhnid,bhnjd->bhnij', k_beta, k) * decay, 0.0)
    eye = jnp.eye(cs, dtype=f32)
    t_mat = lax.linalg.triangular_solve(a_mat + eye, jnp.broadcast_to(eye, a_mat.shape),
                                        left_side=True, lower=True, unit_diagonal=True)
    u = t_mat @ v_beta
    w = t_mat @ (k_beta * jnp.exp(gc)[..., None])
    attn = jnp.where(incl, jnp.einsum('bhnid,bhnjd->bhnij', q, k) * decay, 0.0)
    q_dec = q * jnp.exp(gc)[..., None]
    g_last = gc[..., -1]
    k_dec = k * jnp.exp(g_last[..., None] - gc)[..., None]
    state_decay = jnp.exp(g_last)

    def step(state, inp):
        q_i, k_i, u_i, w_i, a_i, sd_i = inp
        v_new = u_i - w_i @ state
        o_i = q_i @ state + a_i @ v_new
        state = state * sd_i[..., None, None] + jnp.einsum('bhck,bhcv->bhkv', k_i, v_new)
        return state, o_i

    xs = (jnp.moveaxis(q_dec, 2, 0), jnp.moveaxis(k_dec, 2, 0), jnp.moveaxis(u, 2, 0),
          jnp.moveaxis(w, 2, 0), jnp.moveaxis(attn, 2, 0), jnp.moveaxis(state_decay, 2, 0))
    state0 = jnp.zeros((bsz, nh, dh, dh), f32)
    _, o = lax.scan(step, state0, xs)
    o = jnp.moveaxis(o, 0, 2).reshape(bsz, nh, seq, dh)
    o = jnp.swapaxes(o, 1, 2)
    o = o * lax.rsqrt(jnp.mean(o * o, axis=-1, keepdims=True) + NORM_EPS) * norm_w.astype(f32)
    o = o * jax.nn.silu(z)
    return o.reshape(bsz, seq, d).astype(h.dtype) @ w_o


def conv_ffn(h, w_up, conv_w, w_down):
    u = causal_dwconv(h @ w_up, conv_w)
    gate, up = u[..., :FFN_DIM], u[..., FFN_DIM:]
    return (jax.nn.silu(gate) * up) @ w_down


def setup_inputs(seed: int = 0) -> dict:
    key = jax.random.key(seed)
    ks = jax.random.split(key, 24)
    f32 = jnp.float32
    d = D_MODEL

    def dense(k, shape):
        return jax.random.normal(k, shape, f32) * (shape[-2] ** -0.5)

    def gain(k, shape):
        return 1.0 + 0.02 * jax.random.normal(k, shape, f32)

    def conv(k, shape):
        return jax.random.normal(k, shape, f32) * (shape[-2] ** -0.5)

    dt = jnp.exp(jax.random.uniform(ks[14], (N_GDN, GDN_HEADS), f32, math.log(1e-3), math.log(1e-1)))
    return {
        'x': jax.random.normal(ks[0], (BATCH, SEQ, d), f32),
        'mix_norm': gain(ks[1], (DEPTH, d)),
        'ffn_norm': gain(ks[2], (DEPTH, d)),
        'final_norm': gain(ks[3], (d,)),
        'moba_w_qkv': dense(ks[4], (N_MOBA, d, 3 * d)),
        'moba_w_o': dense(ks[5], (N_MOBA, d, d)),
        'sconv_w_in': dense(ks[6], (N_SCONV, d, 3 * d)),
        'sconv_conv': conv(ks[7], (N_SCONV, SCONV_WIDTH, d)),
        'sconv_w_out': dense(ks[8], (N_SCONV, d, d)),
        'gdn_w_in': dense(ks[9], (N_GDN, d, 4 * d + 2 * GDN_HEADS)),
        'gdn_conv': conv(ks[10], (N_GDN, GDN_CONV_WIDTH, 3 * d)),
        'gdn_a_log': jnp.log(jax.random.uniform(ks[11], (N_GDN, GDN_HEADS), f32, 1.0, 16.0)),
        'gdn_dt_bias': dt + jnp.log(-jnp.expm1(-dt)),
        'gdn_norm': gain(ks[12], (N_GDN, GDN_HEAD_DIM)),
        'gdn_w_o': dense(ks[13], (N_GDN, d, d)),
        'ffn_w_up': dense(ks[15], (DEPTH, d, 2 * FFN_DIM)),
        'ffn_conv': conv(ks[16], (DEPTH, FFN_CONV_WIDTH, 2 * FFN_DIM)),
        'ffn_w_down': dense(ks[17], (DEPTH, FFN_DIM, d)),
    }


def reference(x, mix_norm, ffn_norm, final_norm, moba_w_qkv, moba_w_o, sconv_w_in, sconv_conv,
              sconv_w_out, gdn_w_in, gdn_conv, gdn_a_log, gdn_dt_bias, gdn_norm, gdn_w_o,
              ffn_w_up, ffn_conv, ffn_w_down):
    for i in range(DEPTH):
        kind, j = i % N_MIXERS, i // N_MIXERS
        h = rmsnorm(x, mix_norm[i])
        if kind == 0:
            h = moba_attention(h, moba_w_qkv[j], moba_w_o[j])
        elif kind == 1:
            h = short_conv_mixer(h, sconv_w_in[j], sconv_conv[j], sconv_w_out[j])
        else:
            h = gated_deltanet(h, gdn_w_in[j], gdn_conv[j], gdn_a_log[j], gdn_dt_bias[j],
                               gdn_norm[j], gdn_w_o[j])
        x = x + h
        x = x + conv_ffn(rmsnorm(x, ffn_norm[i]), ffn_w_up[i], ffn_conv[i], ffn_w_down[i])
    return rmsnorm(x, final_norm)
```

```python
import math
from contextlib import ExitStack, contextmanager

import numpy as np
import ml_dtypes
import concourse.bass as bass
import concourse.mybir as mybir
from concourse.bass_utils import run_bass_kernel_spmd

F32 = mybir.dt.float32
BF16 = mybir.dt.bfloat16
AF = mybir.ActivationFunctionType
ALU = mybir.AluOpType
AX = mybir.AxisListType
NPBF = ml_dtypes.bfloat16

D = 1024
NH = 8
HD = 128
FFN = 2816
NCORE = 8
EPS = 1e-6
HALO = 128
BIG = 30000.0

SAME_ENGINE_SYNC = True


class Eng:
    def __init__(self, name, e, sem):
        self.name, self.e, self.sem = name, e, sem
        self.cnt = 0
        self.seen = {}


class Buf:
    __slots__ = ("name", "t", "lw", "rds", "sem", "dcnt")

    def __init__(self, name, t=None):
        self.name, self.t = name, t
        self.lw = None
        self.rds = {}
        self.sem = None
        self.dcnt = 0

    def __getitem__(self, idx):
        return self.t[idx]


class Prog:
    def __init__(self, nc, es):
        self.nc, self.es, self.tes = nc, es, es
        self.eng = {}
        for name, e in (("pe", nc.tensor), ("dve", nc.vector), ("act", nc.scalar),
                        ("pool", nc.gpsimd), ("sp", nc.sync)):
            sem = es.enter_context(nc.semaphore("s_" + name))
            self.eng[name] = Eng(name, e, sem)
        self.bufs = []
        self.uid = 0
        self.free_sems = []

    def _name(self, name):
        self.uid += 1
        return "%s_%d" % (name, self.uid)

    def sbuf(self, name, shape, dt):
        name = self._name(name)
        t = self.tes.enter_context(self.nc.sbuf_tensor(name, list(shape), dt))
        b = Buf(name, t)
        self.bufs.append(b)
        return b

    def psum(self, name, shape, dt=F32):
        name = self._name(name)
        t = self.tes.enter_context(self.nc.psum_tensor(name, list(shape), dt))
        b = Buf(name, t)
        self.bufs.append(b)
        return b

    @contextmanager
    def scope(self):
        outer = self.tes
        nb = len(self.bufs)
        with ExitStack() as tes:
            self.tes = tes
            yield
            self.barrier()
        self.tes = outer

    def _wait(self, E, deps):
        best = {}
        for d in deps:
            if d is None:
                continue
            k, sem, v = d
            if k == E.name and (E.name == "pe" or not SAME_ENGINE_SYNC):
                continue
            if E.seen.get(k, 0) >= v:
                continue
            if k not in best or best[k][2] < v:
                best[k] = d
        for k, (kk, sem, v) in best.items():
            E.e.wait_ge(sem, v)
            E.seen[k] = v

    @staticmethod
    def _deps(rd, wr):
        deps = []
        for b in rd:
            deps.append(b.lw)
        for b in wr:
            deps.append(b.lw)
            deps.extend(b.rds.values())
        return deps

    def op(self, en, fn, rd=(), wr=()):
        E = self.eng[en]
        self._wait(E, self._deps(rd, wr))
        ins = fn(E.e)
        E.cnt += 1
        ins.then_inc(E.sem, 1)
        ev = (E.name, E.sem, E.cnt)
        for b in rd:
            b.rds[E.name] = ev
        for b in wr:
            b.lw = ev
            b.rds = {}
        return ins

    def dma(self, qn, out, in_, rd=(), wr=(), **kw):
        Q = self.eng[qn]
        self._wait(Q, self._deps(rd, wr))
        b = (list(wr) + list(rd))[0]
        if b.sem is None:
            b.sem = self.es.enter_context(self.nc.semaphore("d_" + b.name))
        b.dcnt += 1
        Q.e.dma_start(out=out, in_=in_, **kw).then_inc(b.sem, 16)
        ev = ("d_" + b.name, b.sem, 16 * b.dcnt)
        for x in rd:
            x.rds[ev[0]] = ev
        for x in wr:
            x.lw = ev
            x.rds = {}

    def barrier(self, engines=("pe", "dve", "act", "pool", "sp")):
        evs = []
        for E in self.eng.values():
            if E.cnt:
                evs.append((E.name, E.sem, E.cnt))
        for b in self.bufs:
            if b.sem is not None and b.dcnt:
                evs.append(("d_" + b.name, b.sem, 16 * b.dcnt))
        for en in engines:
            E = self.eng[en]
            for (k, sem, v) in evs:
                if k == E.name or E.seen.get(k, 0) >= v:
                    continue
                E.e.wait_ge(sem, v)
                E.seen[k] = v

    def finish(self):
        self.barrier()


class Rot:
    def __init__(self, bufs):
        self.bufs, self.i = bufs, 0

    def get(self):
        b = self.bufs[self.i % len(self.bufs)]
        self.i += 1
        return b


def rot_sbuf(p, name, n, shape, dt):
    return Rot([p.sbuf(name, shape, dt) for _ in range(n)])


def rot_psum(p, name, n, shape, dt=F32):
    return Rot([p.psum(name, shape, dt) for _ in range(n)])


def load_small(p, name, ap, shape, dt=F32, q="sp"):
    b = p.sbuf(name, shape, dt)
    p.dma(q, b[:], ap, wr=[b])
    return b


def load_w(p, wb, w_ap, kch, n, stg):
    wv = w_ap.rearrange("(c p) n -> p c n", p=128)
    step = 2048
    for c in range(kch):
        for n0 in range(0, n, step):
            n1 = min(n, n0 + step)
            s = stg.get()
            p.dma("sp", s[:, 0:n1 - n0], wv[:, c, n0:n1], wr=[s])
            p.op("pool", lambda e: e.tensor_copy(out=wb[:, c, n0:n1], in_=s[:, 0:n1 - n0]), rd=[s], wr=[wb])
    return wb


def rmsnorm_fm(p, xs, N, gs, ones, sq, hs, rstd, ps):
    p.op("act", lambda e: e.activation(out=sq[:, :, 0:N], in_=xs[:, :, 0:N], func=AF.Square), rd=[xs], wr=[sq])
    for c in range(8):
        p.op("pe", lambda e: e.matmul(ps[:, 0:N], lhsT=ones[:], rhs=sq[:, c, 0:N], start=(c == 0), stop=(c == 7)),
             rd=[ones, sq], wr=[ps])
    p.op("act", lambda e: e.activation(out=rstd[:, 0:N], in_=ps[:, 0:N], func=AF.Sqrt, scale=1.0 / D, bias=EPS),
         rd=[ps], wr=[rstd])
    p.op("dve", lambda e: e.reciprocal(out=rstd[:, 0:N], in_=rstd[:, 0:N]), rd=[rstd], wr=[rstd])
    for c in range(8):
        p.op("dve", lambda e: e.scalar_tensor_tensor(out=hs[:, c, 0:N], in0=xs[:, c, 0:N], scalar=gs[:, c:c + 1],
                                                     in1=rstd[:, 0:N], op0=ALU.mult, op1=ALU.mult),
             rd=[xs, gs, rstd], wr=[hs])


def conv_fm(p, ub, N, K, cwb, j, carry, acc, first):
    wk = cwb[:, j, :]
    if first:
        p.op("pool", lambda e: e.memset(ub[:, 0:K - 1], 0.0), wr=[ub])
    else:
        p.op("pool", lambda e: e.tensor_copy(out=ub[:, 0:K - 1], in_=carry[:]), rd=[carry], wr=[ub])
    p.op("pool", lambda e: e.tensor_copy(out=carry[:], in_=ub[:, N:N + K - 1]), rd=[ub], wr=[carry])
    p.op("pool", lambda e: e.tensor_scalar(out=acc[:, 0:N], in0=ub[:, K - 1:K - 1 + N], scalar1=wk[:, K - 1:K],
                                           scalar2=None, op0=ALU.mult), rd=[ub, cwb], wr=[acc])
    for k in range(K - 1):
        p.op("dve", lambda e: e.scalar_tensor_tensor(out=acc[:, 0:N], in0=ub[:, k:k + N], scalar=wk[:, k:k + 1],
                                                     in1=acc[:, 0:N], op0=ALU.mult, op1=ALU.add),
             rd=[ub, acc, cwb], wr=[acc])


def tok_tiles(T, nt):
    tiles = []
    t = 0
    while t < T:
        n = min(nt, T - t)
        tiles.append((t, n))
        t += n
    return tiles


def pass_proj(p, T, xT, g_ap, w_ap, FO, outT, out_dt, nconv=0, K=0, cw_ap=None, scale_chunks=(), scale=1.0, NT=512):
    with p.scope():
        W = p.sbuf("W", [128, 8, FO], BF16)
        with p.scope():
            load_w(p, W, w_ap, 8, FO, rot_sbuf(p, "stg", 2, [128, 2048], F32))
        gs = load_small(p, "gs", g_ap, [128, 8])
        nch = (FO + 127) // 128
        if nconv:
            cw = load_small(p, "cw", cw_ap, [128, nconv, K])
            carry = [p.sbuf("carry", [128, K - 1], F32) for _ in range(nconv)]
            ubs = rot_sbuf(p, "ub", 2, [128, K - 1 + NT], F32)
            accs = rot_sbuf(p, "acc", 2, [128, NT], F32)
        ones = p.sbuf("ones", [128, 128], BF16)
        p.op("dve", lambda e: e.memset(ones[:], 1.0), wr=[ones])
        xsr = rot_sbuf(p, "xs", 2, [128, 8, NT], F32)
        sq = p.sbuf("sq", [128, 8, NT], BF16)
        hsr = rot_sbuf(p, "hs", 2, [128, 8, NT], BF16)
        rstd = p.sbuf("rstd", [128, NT], F32)
        obr = rot_sbuf(p, "ob", 3, [128, NT], out_dt)
        ps_ss = p.psum("ps_ss", [128, 512])
        psr = rot_psum(p, "ps", 3, [128, 512])
        xv = xT.rearrange("(c p) t -> p c t", p=128)
        for ti, (t0, N) in enumerate(tok_tiles(T, NT)):
            xs = xsr.get()
            p.dma("sp", xs[:, :, 0:N], xv[:, :, t0:t0 + N], wr=[xs])
            hs = hsr.get()
            rmsnorm_fm(p, xs, N, gs, ones, sq, hs, rstd, ps_ss)
            for j in range(nch):
                M = min(128, FO - j * 128)
                ps = psr.get()
                for c in range(8):
                    p.op("pe", lambda e: e.matmul(ps[0:M, 0:N], lhsT=W[:, c, j * 128:j * 128 + M], rhs=hs[:, c, 0:N],
                                                  start=(c == 0), stop=(c == 7)), rd=[W, hs], wr=[ps])
                ob = obr.get()
                if j < nconv:
                    ub = ubs.get()
                    acc = accs.get()
                    p.op("act", lambda e: e.activation(out=ub[:, K - 1:K - 1 + N], in_=ps[:, 0:N], func=AF.Copy),
                         rd=[ps], wr=[ub])
                    conv_fm(p, ub, N, K, cw, j, carry[j], acc, ti == 0)
                    p.op("act", lambda e: e.activation(out=ob[:, 0:N], in_=acc[:, 0:N], func=AF.Silu),
                         rd=[acc], wr=[ob])
                else:
                    sc = scale if j in scale_chunks else 1.0
                    p.op("act", lambda e: e.activation(out=ob[0:M, 0:N], in_=ps[0:M, 0:N], func=AF.Copy, scale=sc),
                         rd=[ps], wr=[ob])
                p.dma("pool", outT[j * 128:j * 128 + M, t0:t0 + N], ob[0:M, 0:N], rd=[ob])


def pass_oproj(p, T, xT, oT, o_dt, w_ap, xoutT, NT=512):
    with p.scope():
        W = p.sbuf("Wo", [128, 8, D], BF16)
        with p.scope():
            load_w(p, W, w_ap, 8, D, rot_sbuf(p, "stg", 2, [128, 2048], F32))
        xsr = rot_sbuf(p, "xs", 2, [128, 8, NT], F32)
        osr = rot_sbuf(p, "os", 2, [128, 8, NT], o_dt)
        if o_dt != BF16:
            obr = rot_sbuf(p, "obf", 2, [128, 8, NT], BF16)
        psr = rot_psum(p, "ps", 3, [128, 512])
        xv = xT.rearrange("(c p) t -> p c t", p=128)
        ov = oT.rearrange("(c p) t -> p c t", p=128)
        xov = xoutT.rearrange("(c p) t -> p c t", p=128)
        for ti, (t0, N) in enumerate(tok_tiles(T, NT)):
            xs = xsr.get()
            p.dma("sp", xs[:, :, 0:N], xv[:, :, t0:t0 + N], wr=[xs])
            os_ = osr.get()
            p.dma("sp", os_[:, :, 0:N], ov[:, :, t0:t0 + N], wr=[os_])
            if o_dt != BF16:
                ob = obr.get()
                p.op("pool", lambda e: e.tensor_copy(out=ob[:, :, 0:N], in_=os_[:, :, 0:N]), rd=[os_], wr=[ob])
                os_ = ob
            for fo in range(8):
                ps = psr.get()
                for c in range(8):
                    p.op("pe", lambda e: e.matmul(ps[:, 0:N], lhsT=W[:, c, fo * 128:(fo + 1) * 128], rhs=os_[:, c, 0:N],
                                                  start=(c == 0), stop=(c == 7)), rd=[W, os_], wr=[ps])
                p.op("dve", lambda e: e.tensor_tensor(out=xs[:, fo, 0:N], in0=xs[:, fo, 0:N], in1=ps[:, 0:N], op=ALU.add),
                     rd=[xs, ps], wr=[xs])
            p.dma("pool", xov[:, :, t0:t0 + N], xs[:, :, 0:N], rd=[xs])


def pass_ffn(p, T, xT, g_ap, wup_ap, cw_ap, wdn_ap, xoutT, final_g_ap=None, NT=256):
    NJ = FFN // 128
    with p.scope():
        Wu = p.sbuf("Wu", [128, 8, 2 * FFN], BF16)
        Wd = p.sbuf("Wd", [128, NJ, D], BF16)
        with p.scope():
            stg = rot_sbuf(p, "stg", 2, [128, 2048], F32)
            load_w(p, Wu, wup_ap, 8, 2 * FFN, stg)
            load_w(p, Wd, wdn_ap, NJ, D, stg)
        gs = load_small(p, "gs", g_ap, [128, 8])
        cw = load_small(p, "cw", cw_ap, [128, 2 * NJ, 3])
        if final_g_ap is not None:
            gf = load_small(p, "gf", final_g_ap, [128, 8])
            onesf = p.sbuf("onesf", [128, 128], F32)
            p.op("dve", lambda e: e.memset(onesf[:], 1.0), wr=[onesf])
            sqf = p.sbuf("sqf", [128, 8, NT], F32)
        carry = [p.sbuf("carry", [128, 2], F32) for _ in range(2 * NJ)]
        ones = p.sbuf("ones", [128, 128], BF16)
        p.op("dve", lambda e: e.memset(ones[:], 1.0), wr=[ones])
        xsr = rot_sbuf(p, "xs", 2, [128, 8, NT], F32)
        sq = p.sbuf("sq", [128, 8, NT], BF16)
        hs = p.sbuf("hs", [128, 8, NT], BF16)
        rstd = p.sbuf("rstd", [128, NT], F32)
        act = p.sbuf("actT", [128, NJ, NT], BF16)
        ubr = rot_sbuf(p, "ub", 4, [128, 2 + NT], F32)
        acr = rot_sbuf(p, "acc", 4, [128, NT], F32)
        sgr = rot_sbuf(p, "sg", 2, [128, NT], F32)
        ps_ss = p.psum("ps_ss", [128, 512])
        psr = rot_psum(p, "ps", 4, [128, 512])
        pso = rot_psum(p, "pso", 2, [128, 512])
        xv = xT.rearrange("(c p) t -> p c t", p=128)
        xov = xoutT.rearrange("(c p) t -> p c t", p=128)
        for ti, (t0, N) in enumerate(tok_tiles(T, NT)):
            xs = xsr.get()
            p.dma("sp", xs[:, :, 0:N], xv[:, :, t0:t0 + N], wr=[xs])
            rmsnorm_fm(p, xs, N, gs, ones, sq, hs, rstd, ps_ss)
            for j in range(NJ):
                accs = []
                for half in range(2):
                    col = half * FFN + j * 128
                    ps = psr.get()
                    for c in range(8):
                        p.op("pe", lambda e: e.matmul(ps[:, 0:N], lhsT=Wu[:, c, col:col + 128], rhs=hs[:, c, 0:N],
                                                      start=(c == 0), stop=(c == 7)), rd=[Wu, hs], wr=[ps])
                    ub = ubr.get()
                    acc = acr.get()
                    p.op("act", lambda e: e.activation(out=ub[:, 2:2 + N], in_=ps[:, 0:N], func=AF.Copy),
                         rd=[ps], wr=[ub])
                    conv_fm(p, ub, N, 3, cw, half * NJ + j, carry[half * NJ + j], acc, ti == 0)
                    accs.append(acc)
                sg = sgr.get()
                p.op("act", lambda e: e.activation(out=sg[:, 0:N], in_=accs[0][:, 0:N], func=AF.Silu),
                     rd=[accs[0]], wr=[sg])
                p.op("dve", lambda e: e.tensor_tensor(out=act[:, j, 0:N], in0=sg[:, 0:N], in1=accs[1][:, 0:N],
                                                      op=ALU.mult), rd=[sg, accs[1]], wr=[act])
            for fo in range(8):
                ps = pso.get()
                for j in range(NJ):
                    p.op("pe", lambda e: e.matmul(ps[:, 0:N], lhsT=Wd[:, j, fo * 128:(fo + 1) * 128], rhs=act[:, j, 0:N],
                                                  start=(j == 0), stop=(j == NJ - 1)), rd=[Wd, act], wr=[ps])
                p.op("dve", lambda e: e.tensor_tensor(out=xs[:, fo, 0:N], in0=xs[:, fo, 0:N], in1=ps[:, 0:N], op=ALU.add),
                     rd=[xs, ps], wr=[xs])
            if final_g_ap is not None:
                p.op("act", lambda e: e.activation(out=sqf[:, :, 0:N], in_=xs[:, :, 0:N], func=AF.Square),
                     rd=[xs], wr=[sqf])
                for c in range(8):
                    p.op("pe", lambda e: e.matmul(ps_ss[:, 0:N], lhsT=onesf[:], rhs=sqf[:, c, 0:N], start=(c == 0),
                                                  stop=(c == 7)), rd=[onesf, sqf], wr=[ps_ss])
                p.op("act", lambda e: e.activation(out=rstd[:, 0:N], in_=ps_ss[:, 0:N], func=AF.Sqrt, scale=1.0 / D,
                                                   bias=EPS), rd=[ps_ss], wr=[rstd])
                p.op("dve", lambda e: e.reciprocal(out=rstd[:, 0:N], in_=rstd[:, 0:N]), rd=[rstd], wr=[rstd])
                for c in range(8):
                    p.op("dve", lambda e: e.scalar_tensor_tensor(out=xs[:, c, 0:N], in0=xs[:, c, 0:N],
                                                                 scalar=gf[:, c:c + 1], in1=rstd[:, 0:N],
                                                                 op0=ALU.mult, op1=ALU.mult),
                         rd=[xs, gf, rstd], wr=[xs])
            p.dma("pool", xov[:, :, t0:t0 + N], xs[:, :, 0:N], rd=[xs])


def pass_sconv(p, T, xT, g_ap, win_ap, cw_ap, wout_ap, xoutT, NT=512):
    with p.scope():
        Wi = p.sbuf("Wi", [128, 8, 3 * D], BF16)
        Wo = p.sbuf("Wo", [128, 8, D], BF16)
        with p.scope():
            stg = rot_sbuf(p, "stg", 2, [128, 2048], F32)
            load_w(p, Wi, win_ap, 8, 3 * D, stg)
            load_w(p, Wo, wout_ap, 8, D, stg)
        gs = load_small(p, "gs", g_ap, [128, 8])
        cw = load_small(p, "cw", cw_ap, [128, 8, 3])
        carry = [p.sbuf("carry", [128, 2], F32) for _ in range(8)]
        ones = p.sbuf("ones", [128, 128], BF16)
        p.op("dve", lambda e: e.memset(ones[:], 1.0), wr=[ones])
        xsr = rot_sbuf(p, "xs", 2, [128, 8, NT], F32)
        sq = p.sbuf("sq", [128, 8, NT], BF16)
        hs = p.sbuf("hs", [128, 8, NT], BF16)
        rstd = p.sbuf("rstd", [128, NT], F32)
        ys = p.sbuf("ys", [128, 8, NT], BF16)
        ubr = rot_sbuf(p, "ub", 2, [128, 2 + NT], F32)
        acr = rot_sbuf(p, "acc", 2, [128, NT], F32)
        tmr = rot_sbuf(p, "tm", 2, [128, NT], F32)
        ps_ss = p.psum("ps_ss", [128, 512])
        psr = rot_psum(p, "ps", 6, [128, 512])
        xv = xT.rearrange("(c p) t -> p c t", p=128)
        xov = xoutT.rearrange("(c p) t -> p c t", p=128)
        for ti, (t0, N) in enumerate(tok_tiles(T, NT)):
            xs = xsr.get()
            p.dma("sp", xs[:, :, 0:N], xv[:, :, t0:t0 + N], wr=[xs])
            rmsnorm_fm(p, xs, N, gs, ones, sq, hs, rstd, ps_ss)
            for j in range(8):
                pss = []
                for part in range(3):
                    col = part * D + j * 128
                    ps = psr.get()
                    for c in range(8):
                        p.op("pe", lambda e: e.matmul(ps[:, 0:N], lhsT=Wi[:, c, col:col + 128], rhs=hs[:, c, 0:N],
                                                      start=(c == 0), stop=(c == 7)), rd=[Wi, hs], wr=[ps])
                    pss.append(ps)
                ub = ubr.get()
                acc = acr.get()
                tm = tmr.get()
                p.op("act", lambda e: e.activation(out=tm[:, 0:N], in_=pss[1][:, 0:N], func=AF.Copy),
                     rd=[pss[1]], wr=[tm])
                p.op("dve", lambda e: e.tensor_tensor(out=ub[:, 2:2 + N], in0=tm[:, 0:N], in1=pss[2][:, 0:N],
                                                      op=ALU.mult), rd=[tm, pss[2]], wr=[ub])
                conv_fm(p, ub, N, 3, cw, j, carry[j], acc, ti == 0)
                p.op("dve", lambda e: e.tensor_tensor(out=ys[:, j, 0:N], in0=acc[:, 0:N], in1=pss[0][:, 0:N],
                                                      op=ALU.mult), rd=[acc, pss[0]], wr=[ys])
            for fo in range(8):
                ps = psr.get()
                for c in range(8):
                    p.op("pe", lambda e: e.matmul(ps[:, 0:N], lhsT=Wo[:, c, fo * 128:(fo + 1) * 128], rhs=ys[:, c, 0:N],
                                                  start=(c == 0), stop=(c == 7)), rd=[Wo, ys], wr=[ps])
                p.op("dve", lambda e: e.tensor_tensor(out=xs[:, fo, 0:N], in0=xs[:, fo, 0:N], in1=ps[:, 0:N], op=ALU.add),
                     rd=[xs, ps], wr=[xs])
            p.dma("pool", xov[:, :, t0:t0 + N], xs[:, :, 0:N], rd=[xs])


def moba_attention(p, S, nb, qT, kT, v, oT, ident_ap, esel_ap, cmask_ap, ktab_ap, qrows_ap):
    NB = S // 256
    NQT = S // 512
    NKC = S // 128
    with p.scope():
        ident = load_small(p, "ident", ident_ap, [128, 128], BF16)
        esel = load_small(p, "esel", esel_ap, [128, 64, 128], BF16)
        cmask = load_small(p, "cmask", cmask_ap, [128, 4, 512], BF16)
        ktab = load_small(p, "ktab", ktab_ap, [128, 128], F32)
        onesf = p.sbuf("onesf", [128, 128], F32)
        p.op("dve", lambda e: e.memset(onesf[:], 1.0), wr=[onesf])
        kTs = p.sbuf("kTs", [128, S], BF16)
        qTs = p.sbuf("qTs", [128, S], BF16)
        vs = p.sbuf("vs", [128, NKC, 128], BF16)
        kmf = p.sbuf("kmf", [128, 64], F32)
        kmb = p.sbuf("kmb", [128, 64], BF16)
        bTr = rot_sbuf(p, "biasT", 2, [128, 512], BF16)
        for b_ in bTr.bufs:
            p.op("pool", lambda e: e.memset(b_[:], 0.0), wr=[b_])
            p.dma("sp", b_[64:66, :], qrows_ap, wr=[b_])
        gsbr = rot_sbuf(p, "gsb", 2, [128, 64], F32)
        t8r = rot_sbuf(p, "top8", 2, [128, 8], F32)
        mskr = rot_sbuf(p, "msk", 2, [128, 64], BF16)
        PTr = rot_sbuf(p, "PT", 3, [128, 512], BF16)
        raccr = rot_sbuf(p, "racc", 2, [128, 512], F32)
        rinv = p.sbuf("rinv", [128, 512], F32)
        otr = rot_sbuf(p, "ot", 2, [128, 512], BF16)
        psSr = rot_psum(p, "psS", 3, [128, 512])
        psOr = rot_psum(p, "psO", 2, [128, 512])
        psg = p.psum("psg", [128, 64])
        pst = p.psum("pst", [64, 128], BF16)
        psr_ = p.psum("psr", [128, 512])
        for b in range(nb):
            p.dma("sp", kTs[:], kT[b], wr=[kTs])
            p.dma("sp", qTs[:], qT[b], wr=[qTs])
            p.dma("sp", vs[:], v[b].rearrange("(c p) d -> p c d", p=128), wr=[vs])
            p.op("pool", lambda e: e.memset(kmf[:], 0.0), wr=[kmf])
            p.op("dve", lambda e: e.tensor_reduce(out=kmf[:, 0:NB], in_=kTs[:].rearrange("p (n k) -> p n k", k=256),
                                                  axis=AX.X, op=ALU.add), rd=[kTs], wr=[kmf])
            p.op("dve", lambda e: e.tensor_copy(out=kmb[:], in_=kmf[:]), rd=[kmf], wr=[kmb])
            for qt in range(NQT):
                t0 = qt * 512
                bT = bTr.get()
                for s in range(4):
                    own = 2 * qt + s // 2
                    msk = mskr.get()
                    if own == 0:
                        p.op("pool", lambda e: e.memset(msk[:], 0.0), wr=[msk])
                    else:
                        p.op("pe", lambda e: e.matmul(psg[:], lhsT=qTs[:, t0 + 128 * s:t0 + 128 * s + 128], rhs=kmb[:],
                                                      start=True, stop=True), rd=[qTs, kmb], wr=[psg])
                        gsb = gsbr.get()
                        p.op("pool", lambda e: e.memset(gsb[:], -1e30), wr=[gsb])
                        p.op("act", lambda e: e.activation(out=gsb[:, 0:own], in_=psg[:, 0:own], func=AF.Copy),
                             rd=[psg], wr=[gsb])
                        t8 = t8r.get()
                        p.op("dve", lambda e: e.max(out=t8[:], in_=gsb[:]), rd=[gsb], wr=[t8])
                        p.op("dve", lambda e: e.tensor_scalar(out=msk[:], in0=gsb[:], scalar1=t8[:, 2:3], scalar2=-1.0,
                                                              op0=ALU.is_ge, op1=ALU.add), rd=[gsb, t8], wr=[msk])
                        hi = min(64, own + 2)
                        p.op("pool", lambda e: e.memset(msk[:, own:hi], 0.0), wr=[msk])
                    p.op("pe", lambda e: e.transpose(out=pst[:], in_=msk[:], identity=ident[:]), rd=[msk, ident], wr=[pst])
                    p.op("act", lambda e: e.activation(out=bT[0:64, 128 * s:128 * s + 128], in_=pst[:], func=AF.Copy),
                         rd=[pst], wr=[bT])
                psO = psOr.get()
                racc = raccr.get()
                nkc = 4 * qt + 4
                for kc in range(nkc):
                    n = kc // 2
                    d = kc - 4 * qt
                    psS = psSr.get()
                    p.op("pe", lambda e: e.matmul(psS[:], lhsT=kTs[:, kc * 128:kc * 128 + 128], rhs=qTs[:, t0:t0 + 512],
                                                  start=True, stop=False), rd=[kTs, qTs], wr=[psS])
                    p.op("pe", lambda e: e.matmul(psS[:], lhsT=esel[:, n, :], rhs=bT[:], start=False, stop=(d < 0)),
                         rd=[esel, bT], wr=[psS])
                    if d >= 0:
                        p.op("pe", lambda e: e.matmul(psS[:], lhsT=ident[:], rhs=cmask[:, d, :], start=False, stop=True),
                             rd=[ident, cmask], wr=[psS])
                    PT = PTr.get()
                    m = 4 * qt + 3 - kc
                    p.op("act", lambda e: e.activation(out=PT[:], in_=psS[:], func=AF.Exp, bias=ktab[:, m:m + 1], scale=1.0),
                         rd=[psS, ktab], wr=[PT])
                    p.op("pe", lambda e: e.matmul(psO[:], lhsT=vs[:, kc, :], rhs=PT[:], start=(kc == 0),
                                                  stop=(kc == nkc - 1)), rd=[vs, PT], wr=[psO])
                    if kc == 0:
                        p.op("dve", lambda e: e.tensor_copy(out=racc[:], in_=PT[:]), rd=[PT], wr=[racc])
                    else:
                        p.op("dve", lambda e: e.tensor_tensor(out=racc[:], in0=racc[:], in1=PT[:], op=ALU.add),
                             rd=[racc, PT], wr=[racc])
                p.op("pe", lambda e: e.matmul(psr_[:], lhsT=onesf[:], rhs=racc[:], start=True, stop=True),
                     rd=[onesf, racc], wr=[psr_])
                p.op("dve", lambda e: e.reciprocal(out=rinv[:], in_=psr_[:]), rd=[psr_], wr=[rinv])
                ot = otr.get()
                p.op("dve", lambda e: e.tensor_tensor(out=ot[:], in0=psO[:], in1=rinv[:], op=ALU.mult),
                     rd=[psO, rinv], wr=[ot])
                p.dma("pool", oT[b][:, t0:t0 + 512], ot[:], rd=[ot])


def new_nc():
    return bass.Bass("TRN2", target_bir_lowering=False)


def din(nc, name, shape, dt=F32):
    return nc.dram_tensor(name, list(shape), dt, kind="ExternalInput").ap()


def dout(nc, name, shape, dt=F32):
    return nc.dram_tensor(name, list(shape), dt, kind="ExternalOutput").ap()


def dtmp(nc, name, shape, dt=F32):
    return nc.dram_tensor(name, list(shape), dt, kind="Internal").ap()


def ffn_inputs(nc, tag):
    return dict(g=din(nc, tag + "_g", [128, 8]), wup=din(nc, tag + "_wup", [D, 2 * FFN]),
                cw=din(nc, tag + "_cw", [128, 2 * (FFN // 128), 3]), wdn=din(nc, tag + "_wdn", [FFN, D]))


def build_A(T):
    nc = new_nc()
    xT = din(nc, "xT", [D, T])
    g = din(nc, "g", [128, 8])
    w = din(nc, "w", [D, 3 * D])
    o = dout(nc, "qkvT", [3 * D, T], BF16)
    with ExitStack() as es:
        p = Prog(nc, es)
        pass_proj(p, T, xT, g, w, 3 * D, o, BF16, scale_chunks=range(8), scale=HD ** -0.5)
        p.finish()
    return nc


def build_B(S):
    nc = new_nc()
    qT = din(nc, "qT", [2, 128, S], BF16)
    kT = din(nc, "kT", [2, 128, S], BF16)
    v = din(nc, "v", [2, S, 128], BF16)
    ident = din(nc, "ident", [128, 128], BF16)
    esel = din(nc, "esel", [128, 64, 128], BF16)
    cmask = din(nc, "cmask", [128, 4, 512], BF16)
    ktab = din(nc, "ktab", [128, 128])
    qrows = din(nc, "qrows", [2, 512], BF16)
    oT = dout(nc, "oT", [2, 128, S], BF16)
    with ExitStack() as es:
        p = Prog(nc, es)
        moba_attention(p, S, 2, qT, kT, v, oT, ident, esel, cmask, ktab, qrows)
        p.finish()
    return nc


def build_G(T, final):
    nc = new_nc()
    xT = din(nc, "xT", [D, T])
    oT = din(nc, "oT", [D, T], BF16)
    wo = din(nc, "wo", [D, D])
    f = ffn_inputs(nc, "f")
    gf = din(nc, "gf", [128, 8]) if final else None
    xm = dtmp(nc, "xm", [D, T])
    xo = dout(nc, "xoT", [D, T])
    with ExitStack() as es:
        p = Prog(nc, es)
        pass_oproj(p, T, xT, oT, BF16, wo, xm)
        pass_ffn(p, T, xm, f["g"], f["wup"], f["cw"], f["wdn"], xo, final_g_ap=gf)
        p.finish()
    return nc


def run(nc, in_maps):
    res = run_bass_kernel_spmd(nc, in_maps, core_ids=list(range(NCORE)))
    return res.results


def shard_T(full, S):
    TQ = S // 4
    outs = []
    for c in range(NCORE):
        b, qd = divmod(c, 4)
        lo = qd * TQ - HALO
        if lo < 0:
            blk = np.concatenate([np.zeros((HALO, full.shape[2]), full.dtype), full[b, 0:TQ]], axis=0)
        else:
            blk = full[b, lo:lo + HALO + TQ]
        outs.append(np.ascontiguousarray(blk.T))
    return outs


def unshard_T(parts, S):
    TQ = S // 4
    F = parts[0].shape[0]
    full = np.empty((2, S, F), parts[0].dtype)
    for c in range(NCORE):
        b, qd = divmod(c, 4)
        full[b, qd * TQ:(qd + 1) * TQ] = parts[c][:, HALO:].T
    return full


def vec128(v):
    return np.ascontiguousarray(v.reshape(-1, 128).T)


def conv128(w):
    K, C = w.shape
    return np.ascontiguousarray(w.T.reshape(C // 128, 128, K).transpose(1, 0, 2))


def ffn_maps(tag, g, wup, cw, wdn):
    return {tag + "_g": vec128(g), tag + "_wup": wup, tag + "_cw": conv128(cw), tag + "_wdn": wdn}


def moba_consts(h):
    slope = 2.0 ** (-(h + 1))
    ident = np.eye(128, dtype=np.float32).astype(NPBF)
    esel = np.zeros((128, 64, 128), np.float32)
    for n in range(64):
        esel[n, n, :] = BIG
    esel[64:66, :, :] = 1.0
    j = np.arange(128)[:, None]
    i = np.arange(512)[None, :]
    cmask = np.zeros((128, 4, 512), np.float32)
    for d in range(4):
        cmask[:, d, :] = np.where(128 * d + j <= i, 0.0, -BIG)
    m = np.arange(128)[None, :]
    ktab = (slope * (j - 127 - 128 * m)).astype(np.float32)
    r = 511 - np.arange(512)
    qrows = np.stack([slope * (r // 4 * 4), slope * (r % 4)]).astype(np.float32)
    return dict(ident=ident, esel=esel.astype(NPBF), cmask=cmask.astype(NPBF), ktab=ktab, qrows=qrows.astype(NPBF))


_CACHE = {}


def cached(key, fn):
    if key not in _CACHE:
        _CACHE[key] = fn()
    return _CACHE[key]


def moba_layer_mix(x_full, S, g, wqkv):
    T = HALO + S // 4
    ncA = cached(("A", T), lambda: build_A(T))
    xs = shard_T(x_full, S)
    resA = run(ncA, [{"xT": xs[c], "g": vec128(g), "w": wqkv} for c in range(NCORE)])
    qkv = unshard_T([r["qkvT"] for r in resA], S)
    ncB = cached(("B", S), lambda: build_B(S))
    maps = []
    for h in range(NH):
        m = dict(qT=np.ascontiguousarray(qkv[:, :, h * 128:(h + 1) * 128].transpose(0, 2, 1)),
                 kT=np.ascontiguousarray(qkv[:, :, D + h * 128:D + (h + 1) * 128].transpose(0, 2, 1)),
                 v=np.ascontiguousarray(qkv[:, :, 2 * D + h * 128:2 * D + (h + 1) * 128]))
        m.update(moba_consts(h))
        maps.append(m)
    resB = run(ncB, maps)
    o_full = np.concatenate([r["oT"].transpose(0, 2, 1) for r in resB], axis=2)
    return xs, shard_T(o_full, S)


def gdn_scan(p, S, q, k, v, z, atab, btab, alog_ap, dtb_ap, normw_ap, U_ap, SL_ap, SU_ap, UI_ap, id64_ap, y):
    NCH = S // 64
    G = 8
    NG = NCH // G
    with p.scope():
        U = load_small(p, "U", U_ap, [64, 64])
        SL = load_small(p, "SL", SL_ap, [64, 64])
        SU = load_small(p, "SU", SU_ap, [64, 64])
        UI = load_small(p, "UI", UI_ap, [64, 64])
        id64 = load_small(p, "id64", id64_ap, [64, 64])
        normw = load_small(p, "normw", normw_ap, [64, 128])
        alog = load_small(p, "alog", alog_ap, [128, 1])
        dtb = load_small(p, "dtb", dtb_ap, [128, 1])
        ones64 = p.sbuf("ones64", [64, 128], F32)
        p.op("dve", lambda e: e.memset(ones64[:], 1.0), wr=[ones64])
        nega = p.sbuf("nega", [128, 1], F32)
        p.op("act", lambda e: e.activation(out=nega[:], in_=alog[:], func=AF.Exp), rd=[alog], wr=[nega])
        p.op("dve", lambda e: e.tensor_scalar(out=nega[:], in0=nega[:], scalar1=-1.0, scalar2=None, op0=ALU.mult),
             rd=[nega], wr=[nega])
        gtab, betab = [], []
        for b in range(2):
            at = load_small(p, "at", atab[b], [64, NCH])
            bt = load_small(p, "bt", btab[b], [64, NCH])
            xx = p.sbuf("xx", [64, NCH], F32)
            ax = p.sbuf("ax", [64, NCH], F32)
            gt = p.sbuf("gt", [64, NCH], F32)
            be = p.sbuf("be", [64, NCH], F32)
            p.op("dve", lambda e: e.tensor_scalar(out=xx[:], in0=at[:], scalar1=dtb[0:64, 0:1], scalar2=None, op0=ALU.add),
                 rd=[at, dtb], wr=[xx])
            p.op("dve", lambda e: e.tensor_scalar(out=ax[:], in0=xx[:], scalar1=-1.0, scalar2=None, op0=ALU.mult),
                 rd=[xx], wr=[ax])
            p.op("dve", lambda e: e.tensor_tensor(out=ax[:], in0=ax[:], in1=xx[:], op=ALU.min), rd=[ax, xx], wr=[ax])
            p.op("act", lambda e: e.activation(out=ax[:], in_=ax[:], func=AF.Exp), rd=[ax], wr=[ax])
            p.op("act", lambda e: e.activation(out=ax[:], in_=ax[:], func=AF.Ln, bias=1.0, scale=1.0), rd=[ax], wr=[ax])
            p.op("dve", lambda e: e.tensor_scalar(out=xx[:], in0=xx[:], scalar1=0.0, scalar2=None, op0=ALU.max),
                 rd=[xx], wr=[xx])
            p.op("dve", lambda e: e.tensor_tensor(out=xx[:], in0=xx[:], in1=ax[:], op=ALU.add), rd=[xx, ax], wr=[xx])
            p.op("dve", lambda e: e.tensor_scalar(out=gt[:], in0=xx[:], scalar1=nega[0:64, 0:1], scalar2=None, op0=ALU.mult),
                 rd=[xx, nega], wr=[gt])
            p.op("act", lambda e: e.activation(out=be[:], in_=bt[:], func=AF.Exp, scale=-1.0), rd=[bt], wr=[be])
            p.op("dve", lambda e: e.tensor_scalar(out=be[:], in0=be[:], scalar1=1.0, scalar2=None, op0=ALU.add),
                 rd=[be], wr=[be])
            p.op("dve", lambda e: e.reciprocal(out=be[:], in_=be[:]), rd=[be], wr=[be])
            gtab.append(gt)
            betab.append(be)

        rots = {}

        def Tm(name, shape, n=2):
            if name not in rots:
                rots[name] = rot_sbuf(p, name, n, shape, F32)
            return rots[name].get()

        PS = rot_psum(p, "ps", 7, [128, 512])
        Sst = [[p.sbuf("S", [128, 128], F32) for _ in range(2)] for _ in range(2)]
        for b in range(2):
            p.op("pool", lambda e: e.memset(Sst[b][0][:], 0.0), wr=[Sst[b][0]])
        gq = [rot_sbuf(p, "gq", 2, [64, G, 128], F32) for _ in range(2)]
        gk = [rot_sbuf(p, "gk", 2, [64, G, 128], F32) for _ in range(2)]
        gv = [rot_sbuf(p, "gv", 2, [64, G, 128], F32) for _ in range(2)]
        gz = [rot_sbuf(p, "gz", 2, [64, G, 128], F32) for _ in range(2)]
        gy = [rot_sbuf(p, "gy", 2, [64, G, 128], F32) for _ in range(2)]

        def mm(out_ap, ps, lhsT, rhs, rd, start=True, stop=True):
            p.op("pe", lambda e: e.matmul(out_ap, lhsT=lhsT, rhs=rhs, start=start, stop=stop), rd=rd, wr=[ps])

        def evac(eng, name, shape, ps, ps_ap):
            t = Tm(name, shape)
            if eng == "act":
                p.op("act", lambda e: e.activation(out=t[:], in_=ps_ap, func=AF.Copy), rd=[ps], wr=[t])
            else:
                p.op("dve", lambda e: e.tensor_copy(out=t[:], in_=ps_ap), rd=[ps], wr=[t])
            return t

        def tscal(eng, name, shape, src, src_ap, sc, sc_ap, s2=None):
            t = Tm(name, shape)
            if s2 is None:
                p.op(eng, lambda e: e.tensor_scalar(out=t[:], in0=src_ap, scalar1=sc_ap, scalar2=None, op0=ALU.mult),
                     rd=[src, sc], wr=[t])
            else:
                p.op(eng, lambda e: e.tensor_scalar(out=t[:], in0=src_ap, scalar1=sc_ap, scalar2=s2, op0=ALU.mult,
                                                    op1=ALU.mult), rd=[src, sc], wr=[t])
            return t

        def rnorm(src, src_ap, scale_in):
            junk = Tm("junk", [64, 128])
            ss = Tm("ss", [64, 1], 4)
            p.op("dve", lambda e: e.scalar_tensor_tensor(out=junk[:], in0=src_ap, scalar=1.0, in1=src_ap, op0=ALU.mult,
                                                         op1=ALU.mult, accum_out=ss[:]), rd=[src], wr=[junk, ss])
            lg = Tm("lg", [64, 1], 4)
            p.op("act", lambda e: e.activation(out=lg[:], in_=ss[:], func=AF.Ln, bias=EPS, scale=scale_in), rd=[ss], wr=[lg])
            r = Tm("rr", [64, 1], 4)
            p.op("act", lambda e: e.activation(out=r[:], in_=lg[:], func=AF.Exp, scale=-0.5), rd=[lg], wr=[r])
            return r

        def chunk(b, ci, qg, kg, vg, zg, yg, gi):
            gtb, beb = gtab[b], betab[b]
            gcol = gtb[:, ci:ci + 1]
            bcol = beb[:, ci:ci + 1]
            qa, ka, va, za = qg[:, gi, :], kg[:, gi, :], vg[:, gi, :], zg[:, gi, :]
            rq = rnorm(qg, qa, 1.0)
            qn = tscal("dve", "qn", [64, 128], qg, qa, rq, rq[:, 0:1], s2=HD ** -0.5)
            rk = rnorm(kg, ka, 1.0)
            kn = tscal("dve", "kn", [64, 128], kg, ka, rk, rk[:, 0:1])
            kb = tscal("pool", "kb", [64, 128], kn, kn[:], beb, bcol)
            vb = tscal("pool", "vb", [64, 128], vg, va, beb, bcol)
            ps = PS.get()
            mm(ps[0:64, 0:1], ps, U[:], gcol, [U, gtb])
            GC = evac("act", "GC", [64, 1], ps, ps[0:64, 0:1])
            gb = tscal("pool", "gb", [64, 128], ones64, ones64[:], gtb, gcol)
            ps = PS.get()
            mm(ps[:, 0:64], ps, gb[:], U[:], [gb, U])
            GR = evac("act", "GR", [128, 64], ps, ps[:, 0:64])
            egc = Tm("egc", [64, 1], 4)
            p.op("act", lambda e: e.activation(out=egc[:], in_=GC[:], func=AF.Exp), rd=[GC], wr=[egc])
            sdbc = Tm("sdbc", [128, 1], 4)
            p.op("act", lambda e: e.activation(out=sdbc[:], in_=GR[:, 63:64], func=AF.Exp), rd=[GR], wr=[sdbc])
            kdsc = Tm("kdsc", [64, 1], 4)
            p.op("act", lambda e: e.activation(out=kdsc[:], in_=GC[:], func=AF.Exp, scale=-1.0, bias=GR[0:64, 63:64]),
                 rd=[GC, GR], wr=[kdsc])
            dm = Tm("dm", [64, 64])
            p.op("dve", lambda e: e.tensor_scalar(out=dm[:], in0=GR[0:64, :], scalar1=GC[:, 0:1], scalar2=0.0,
                                                  op0=ALU.subtract, op1=ALU.min), rd=[GR, GC], wr=[dm])
            decT = Tm("decT", [64, 64])
            p.op("act", lambda e: e.activation(out=decT[:], in_=dm[:], func=AF.Exp), rd=[dm], wr=[decT])
            dx = Tm("dx", [64, 64])
            p.op("dve", lambda e: e.tensor_scalar(out=dx[:], in0=GR[0:64, :], scalar1=GC[:, 0:1], scalar2=0.0,
                                                  op0=ALU.subtract, op1=ALU.max), rd=[GR, GC], wr=[dx])
            dec = Tm("dec", [64, 64])
            p.op("act", lambda e: e.activation(out=dec[:], in_=dx[:], func=AF.Exp, scale=-1.0), rd=[dx], wr=[dec])
            dSU = Tm("dSU", [64, 64])
            p.op("pool", lambda e: e.tensor_tensor(out=dSU[:], in0=decT[:], in1=SU[:], op=ALU.mult), rd=[decT, SU], wr=[dSU])
            dUI = Tm("dUI", [64, 64])
            p.op("pool", lambda e: e.tensor_tensor(out=dUI[:], in0=decT[:], in1=UI[:], op=ALU.mult), rd=[decT, UI], wr=[dUI])
            dSL = Tm("dSL", [64, 64])
            p.op("pool", lambda e: e.tensor_tensor(out=dSL[:], in0=dec[:], in1=SL[:], op=ALU.mult), rd=[dec, SL], wr=[dSL])
            kbg = tscal("pool", "kbg", [64, 128], kb, kb[:], egc, egc[:, 0:1])
            kd = tscal("pool", "kd", [64, 128], kn, kn[:], kdsc, kdsc[:, 0:1])
            qd = tscal("pool", "qd", [64, 128], qn, qn[:], egc, egc[:, 0:1])
            tr = {}
            for nm, src in (("knT", kn), ("kbT", kb), ("qnT", qn), ("qdT", qd)):
                ps = PS.get()
                p.op("pe", lambda e: e.transpose(out=ps[:, 0:64], in_=src[:], identity=id64[:]), rd=[src, id64], wr=[ps])
                tr[nm] = evac("act" if nm in ("knT", "qnT") else "dve", nm, [128, 64], ps, ps[:, 0:64])
            knT, kbT, qnT, qdT = tr["knT"], tr["kbT"], tr["qnT"], tr["qdT"]

            def masked(nm, lhsT, rhs, msk):
                ps = PS.get()
                mm(ps[0:64, 0:64], ps, lhsT[:], rhs[:], [lhsT, rhs])
                t = Tm(nm, [64, 64])
                p.op("dve", lambda e: e.tensor_tensor(out=t[:], in0=ps[0:64, 0:64], in1=msk[:], op=ALU.mult),
                     rd=[ps, msk], wr=[t])
                return t

            A = masked("A", kbT, knT, dSL)
            B = masked("B", knT, kbT, dSU)
            attnT = masked("attnT", knT, qnT, dUI)
            R = Tm("R", [64, 64], 3)
            p.op("pool", lambda e: e.tensor_tensor(out=R[:], in0=id64[:], in1=B[:], op=ALU.subtract), rd=[id64, B], wr=[R])
            Pp, Qp = A, B
            for kk in range(1, 6):
                ps = PS.get()
                mm(ps[0:64, 0:64], ps, Qp[:], Pp[:], [Qp, Pp])
                Pk = evac("act", "Pk", [64, 64], ps, ps[0:64, 0:64])
                if kk < 5:
                    ps = PS.get()
                    mm(ps[0:64, 0:64], ps, Pp[:], Qp[:], [Pp, Qp])
                    Qk = evac("dve", "Qk", [64, 64], ps, ps[0:64, 0:64])
                ps = PS.get()
                mm(ps[0:64, 0:64], ps, Pk[:], R[:], [Pk, R])
                Rn = Tm("R", [64, 64], 3)
                p.op("dve", lambda e: e.tensor_tensor(out=Rn[:], in0=R[:], in1=ps[0:64, 0:64], op=ALU.add),
                     rd=[R, ps], wr=[Rn])
                R = Rn
                Pp = Pk
                if kk < 5:
                    Qp = Qk
            ps = PS.get()
            mm(ps[0:64, 0:128], ps, R[:], vb[:], [R, vb])
            u = evac("act", "u", [64, 128], ps, ps[0:64, 0:128])
            ps = PS.get()
            mm(ps[:, 0:64], ps, kbg[:], R[:], [kbg, R])
            wT = evac("act", "wT", [128, 64], ps, ps[:, 0:64])
            Sc = Sst[b][ci % 2]
            Sn = Sst[b][(ci + 1) % 2]
            ps = PS.get()
            mm(ps[0:64, 0:128], ps, wT[:], Sc[:], [wT, Sc])
            vnew = Tm("vnew", [64, 128])
            p.op("dve", lambda e: e.tensor_tensor(out=vnew[:], in0=u[:], in1=ps[0:64, 0:128], op=ALU.subtract),
                 rd=[u, ps], wr=[vnew])
            ps3 = PS.get()
            mm(ps3[:, 0:128], ps3, kd[:], vnew[:], [kd, vnew])
            p.op("dve", lambda e: e.scalar_tensor_tensor(out=Sn[:], in0=Sc[:], scalar=sdbc[:, 0:1], in1=ps3[:, 0:128],
                                                         op0=ALU.mult, op1=ALU.add), rd=[Sc, sdbc, ps3], wr=[Sn])
            ps2 = PS.get()
            mm(ps2[0:64, 0:128], ps2, qdT[:], Sc[:], [qdT, Sc], start=True, stop=False)
            mm(ps2[0:64, 0:128], ps2, attnT[:], vnew[:], [attnT, vnew], start=False, stop=True)
            o = evac("act", "o", [64, 128], ps2, ps2[0:64, 0:128])
            ro = rnorm(o, o[:], 1.0 / HD)
            y1 = tscal("pool", "y1", [64, 128], o, o[:], ro, ro[:, 0:1])
            y2 = Tm("y2", [64, 128])
            p.op("pool", lambda e: e.tensor_tensor(out=y2[:], in0=y1[:], in1=normw[:], op=ALU.mult), rd=[y1, normw], wr=[y2])
            ez = Tm("ez", [64, 128])
            p.op("act", lambda e: e.activation(out=ez[:], in_=za, func=AF.Exp, scale=-1.0), rd=[zg], wr=[ez])
            p.op("pool", lambda e: e.tensor_scalar(out=ez[:], in0=ez[:], scalar1=1.0, scalar2=None, op0=ALU.add),
                 rd=[ez], wr=[ez])
            p.op("dve", lambda e: e.reciprocal(out=ez[:], in_=ez[:]), rd=[ez], wr=[ez])
            p.op("pool", lambda e: e.tensor_tensor(out=ez[:], in0=ez[:], in1=za, op=ALU.mult), rd=[ez, zg], wr=[ez])
            p.op("dve", lambda e: e.tensor_tensor(out=yg[:, gi, :], in0=y2[:], in1=ez[:], op=ALU.mult), rd=[y2, ez], wr=[yg])

        for g in range(NG):
            cur = []
            for b in range(2):
                sl = slice(g * G * 64, (g + 1) * G * 64)
                bufs = []
                for pool_, src in ((gq, q), (gk, k), (gv, v), (gz, z)):
                    t = pool_[b].get()
                    p.dma("sp", t[:], src[b][sl, :].rearrange("(g p) d -> p g d", p=64), wr=[t])
                    bufs.append(t)
                bufs.append(gy[b].get())
                cur.append(bufs)
            for gi in range(G):
                for b in range(2):
                    qg, kg, vg, zg, yg = cur[b]
                    chunk(b, g * G + gi, qg, kg, vg, zg, yg, gi)
            for b in range(2):
                sl = slice(g * G * 64, (g + 1) * G * 64)
                yg = cur[b][4]
                p.dma("pool", y[b][sl, :].rearrange("(g p) d -> p g d", p=64), yg[:], rd=[yg])


def build_D(S):
    nc = new_nc()
    q = din(nc, "q", [2, S, 128])
    k = din(nc, "k", [2, S, 128])
    v = din(nc, "v", [2, S, 128])
    z = din(nc, "z", [2, S, 128])
    atab = din(nc, "atab", [2, 64, S // 64])
    btab = din(nc, "btab", [2, 64, S // 64])
    alog = din(nc, "alog", [128, 1])
    dtb = din(nc, "dtb", [128, 1])
    normw = din(nc, "normw", [64, 128])
    U = din(nc, "U", [64, 64])
    SL = din(nc, "SL", [64, 64])
    SU = din(nc, "SU", [64, 64])
    UI = din(nc, "UI", [64, 64])
    id64 = din(nc, "id64", [64, 64])
    y = dout(nc, "y", [2, S, 128])
    with ExitStack() as es:
        p = Prog(nc, es)
        gdn_scan(p, S, q, k, v, z, atab, btab, alog, dtb, normw, U, SL, SU, UI, id64, y)
        p.finish()
    return nc


def build_C(T):
    nc = new_nc()
    xT = din(nc, "xT", [D, T])
    oT = din(nc, "oT", [D, T], BF16)
    wo = din(nc, "wo", [D, D])
    f0 = ffn_inputs(nc, "f0")
    g1 = din(nc, "g1", [128, 8])
    swin = din(nc, "swin", [D, 3 * D])
    scw = din(nc, "scw", [128, 8, 3])
    swout = din(nc, "swout", [D, D])
    f1 = ffn_inputs(nc, "f1")
    g2 = din(nc, "g2", [128, 8])
    gwin = din(nc, "gwin", [D, 4 * D + 16])
    gcw = din(nc, "gcw", [128, 24, 4])
    xm0 = dtmp(nc, "xm0", [D, T])
    x1 = dtmp(nc, "x1", [D, T])
    xm1 = dtmp(nc, "xm1", [D, T])
    x2 = dout(nc, "x2T", [D, T])
    gp = dout(nc, "gpT", [4 * D + 16, T])
    with ExitStack() as es:
        p = Prog(nc, es)
        pass_oproj(p, T, xT, oT, BF16, wo, xm0)
        pass_ffn(p, T, xm0, f0["g"], f0["wup"], f0["cw"], f0["wdn"], x1)
        pass_sconv(p, T, x1, g1, swin, scw, swout, xm1)
        pass_ffn(p, T, xm1, f1["g"], f1["wup"], f1["cw"], f1["wdn"], x2)
        pass_proj(p, T, x2, g2, gwin, 4 * D + 16, gp, F32, nconv=24, K=4, cw_ap=gcw)
        p.finish()
    return nc


def build_E(T):
    nc = new_nc()
    xT = din(nc, "xT", [D, T])
    yT = din(nc, "yT", [D, T])
    wo = din(nc, "wo", [D, D])
    f2 = ffn_inputs(nc, "f2")
    g3 = din(nc, "g3", [128, 8])
    w3 = din(nc, "w3", [D, 3 * D])
    xm = dtmp(nc, "xm", [D, T])
    x3 = dout(nc, "x3T", [D, T])
    qkv = dout(nc, "qkvT", [3 * D, T], BF16)
    with ExitStack() as es:
        p = Prog(nc, es)
        pass_oproj(p, T, xT, yT, F32, wo, xm)
        pass_ffn(p, T, xm, f2["g"], f2["wup"], f2["cw"], f2["wdn"], x3)
        pass_proj(p, T, x3, g3, w3, 3 * D, qkv, BF16, scale_chunks=range(8), scale=HD ** -0.5)
        p.finish()
    return nc


def gdn_consts():
    i = np.arange(64)
    U = (i[:, None] <= i[None, :]).astype(np.float32)
    SL = (i[:, None] > i[None, :]).astype(np.float32)
    SU = (i[None, :] > i[:, None]).astype(np.float32)
    UI = (i[None, :] >= i[:, None]).astype(np.float32)
    return dict(U=U, SL=SL, SU=SU, UI=UI, id64=np.eye(64, dtype=np.float32))


def moba_attn(qkv, S):
    ncB = cached(("B", S), lambda: build_B(S))
    maps = []
    for h in range(NH):
        m = dict(qT=np.ascontiguousarray(qkv[:, :, h * 128:(h + 1) * 128].transpose(0, 2, 1)),
                 kT=np.ascontiguousarray(qkv[:, :, D + h * 128:D + (h + 1) * 128].transpose(0, 2, 1)),
                 v=np.ascontiguousarray(qkv[:, :, 2 * D + h * 128:2 * D + (h + 1) * 128]))
        m.update(moba_consts(h))
        maps.append(m)
    resB = run(ncB, maps)
    return np.concatenate([r["oT"].transpose(0, 2, 1) for r in resB], axis=2)


def forward(inp, S, dbg=None):
    f32 = np.float32
    inp = {k_: np.asarray(v_, dtype=f32) for k_, v_ in inp.items()}
    T = HALO + S // 4
    x = inp["x"]

    def fm(tag, i):
        return ffn_maps(tag, inp["ffn_norm"][i], inp["ffn_w_up"][i], inp["ffn_conv"][i], inp["ffn_w_down"][i])

    ncA = cached(("A", T), lambda: build_A(T))
    xs = shard_T(x, S)
    resA = run(ncA, [{"xT": xs[c], "g": vec128(inp["mix_norm"][0]), "w": inp["moba_w_qkv"][0]} for c in range(NCORE)])
    qkv = unshard_T([r["qkvT"] for r in resA], S)
    os_ = shard_T(moba_attn(qkv, S), S)
    ncC = cached(("C", T), lambda: build_C(T))
    maps = []
    for c in range(NCORE):
        m = {"xT": xs[c], "oT": os_[c], "wo": inp["moba_w_o"][0], "g1": vec128(inp["mix_norm"][1]),
             "swin": inp["sconv_w_in"][0], "scw": conv128(inp["sconv_conv"][0]), "swout": inp["sconv_w_out"][0],
             "g2": vec128(inp["mix_norm"][2]), "gwin": inp["gdn_w_in"][0], "gcw": conv128(inp["gdn_conv"][0])}
        m.update(fm("f0", 0))
        m.update(fm("f1", 1))
        maps.append(m)
    resC = run(ncC, maps)
    x2 = unshard_T([r["x2T"] for r in resC], S)
    gp = unshard_T([r["gpT"] for r in resC], S)
    if dbg is not None:
        dbg["ffn1"] = x2
        dbg["gp"] = gp
    ncD = cached(("D", S), lambda: build_D(S))
    NCH = S // 64
    gc_ = gdn_consts()
    maps = []
    for h in range(NH):
        m = dict(q=np.ascontiguousarray(gp[:, :, h * 128:(h + 1) * 128]),
                 k=np.ascontiguousarray(gp[:, :, D + h * 128:D + (h + 1) * 128]),
                 v=np.ascontiguousarray(gp[:, :, 2 * D + h * 128:2 * D + (h + 1) * 128]),
                 z=np.ascontiguousarray(gp[:, :, 3 * D + h * 128:3 * D + (h + 1) * 128]),
                 btab=np.ascontiguousarray(gp[:, :, 4 * D + h].reshape(2, NCH, 64).transpose(0, 2, 1)),
                 atab=np.ascontiguousarray(gp[:, :, 4 * D + NH + h].reshape(2, NCH, 64).transpose(0, 2, 1)),
                 alog=np.full((128, 1), inp["gdn_a_log"][0][h], f32),
                 dtb=np.full((128, 1), inp["gdn_dt_bias"][0][h], f32),
                 normw=np.ascontiguousarray(np.broadcast_to(inp["gdn_norm"][0][None, :], (64, 128))))
        m.update(gc_)
        maps.append(m)
    resD = run(ncD, maps)
    yfull = np.concatenate([r["y"] for r in resD], axis=2)
    if dbg is not None:
        dbg["y"] = yfull
    ncE = cached(("E", T), lambda: build_E(T))
    x2s = shard_T(x2, S)
    ys = shard_T(yfull, S)
    maps = []
    for c in range(NCORE):
        m = {"xT": x2s[c], "yT": ys[c], "wo": inp["gdn_w_o"][0], "g3": vec128(inp["mix_norm"][3]),
             "w3": inp["moba_w_qkv"][1]}
        m.update(fm("f2", 2))
        maps.append(m)
    resE = run(ncE, maps)
    x3 = unshard_T([r["x3T"] for r in resE], S)
    qkv = unshard_T([r["qkvT"] for r in resE], S)
    if dbg is not None:
        dbg["ffn2"] = x3
    os_ = shard_T(moba_attn(qkv, S), S)
    ncG = cached(("G", T), lambda: build_G(T, True))
    x3s = shard_T(x3, S)
    maps = []
    for c in range(NCORE):
        m = {"xT": x3s[c], "oT": os_[c], "wo": inp["moba_w_o"][1], "gf": vec128(inp["final_norm"])}
        m.update(fm("f", 3))
        maps.append(m)
    resG = run(ncG, maps)
    return unshard_T([r["xoT"] for r in resG], S)


def kernel(**inputs):
    S = inputs["x"].shape[1]
    return forward(inputs, S).astype(np.float32)
```

```python
import math
from contextlib import ExitStack, contextmanager

import numpy as np
import ml_dtypes
import concourse.bass as bass
import concourse.mybir as mybir
from concourse.bass_utils import run_bass_kernel_spmd

F32 = mybir.dt.float32
BF16 = mybir.dt.bfloat16
AF = mybir.ActivationFunctionType
ALU = mybir.AluOpType
AX = mybir.AxisListType
NPBF = ml_dtypes.bfloat16

D = 1024
NH = 8
HD = 128
FFN = 2816
NCORE = 8
EPS = 1e-6
HALO = 128
BIG = 30000.0

SAME_ENGINE_SYNC = True


class Eng:
    def __init__(self, name, e, sem):
        self.name, self.e, self.sem = name, e, sem
        self.cnt = 0
        self.seen = {}


class Buf:
    __slots__ = ("name", "t", "lw", "rds", "sem", "dcnt")

    def __init__(self, name, t=None):
        self.name, self.t = name, t
        self.lw = None
        self.rds = {}
        self.sem = None
        self.dcnt = 0

    def __getitem__(self, idx):
        return self.t[idx]


class Prog:
    def __init__(self, nc, es):
        self.nc, self.es, self.tes = nc, es, es
        self.eng = {}
        for name, e in (("pe", nc.tensor), ("dve", nc.vector), ("act", nc.scalar),
                        ("pool", nc.gpsimd), ("sp", nc.sync)):
            sem = es.enter_context(nc.semaphore("s_" + name))
            self.eng[name] = Eng(name, e, sem)
        self.bufs = []
        self.uid = 0
        self.free_sems = []

    def _name(self, name):
        self.uid += 1
        return "%s_%d" % (name, self.uid)

    def sbuf(self, name, shape, dt):
        name = self._name(name)
        t = self.tes.enter_context(self.nc.sbuf_tensor(name, list(shape), dt))
        b = Buf(name, t)
        self.bufs.append(b)
        return b

    def psum(self, name, shape, dt=F32):
        name = self._name(name)
        t = self.tes.enter_context(self.nc.psum_tensor(name, list(shape), dt))
        b = Buf(name, t)
        self.bufs.append(b)
        return b

    @contextmanager
    def scope(self):
        outer = self.tes
        nb = len(self.bufs)
        with ExitStack() as tes:
            self.tes = tes
            yield
            self.barrier()
        self.tes = outer

    def _wait(self, E, deps):
        best = {}
        for d in deps:
            if d is None:
                continue
            k, sem, v = d
            if k == E.name and (E.name == "pe" or not SAME_ENGINE_SYNC):
                continue
            if E.seen.get(k, 0) >= v:
                continue
            if k not in best or best[k][2] < v:
                best[k] = d
        for k, (kk, sem, v) in best.items():
            E.e.wait_ge(sem, v)
            E.seen[k] = v

    @staticmethod
    def _deps(rd, wr):
        deps = []
        for b in rd:
            deps.append(b.lw)
        for b in wr:
            deps.append(b.lw)
            deps.extend(b.rds.values())
        return deps

    def op(self, en, fn, rd=(), wr=()):
        E = self.eng[en]
        self._wait(E, self._deps(rd, wr))
        ins = fn(E.e)
        E.cnt += 1
        ins.then_inc(E.sem, 1)
        ev = (E.name, E.sem, E.cnt)
        for b in rd:
            b.rds[E.name] = ev
        for b in wr:
            b.lw = ev
            b.rds = {}
        return ins

    def dma(self, qn, out, in_, rd=(), wr=(), **kw):
        Q = self.eng[qn]
        self._wait(Q, self._deps(rd, wr))
        b = (list(wr) + list(rd))[0]
        if b.sem is None:
            b.sem = self.es.enter_context(self.nc.semaphore("d_" + b.name))
        b.dcnt += 1
        Q.e.dma_start(out=out, in_=in_, **kw).then_inc(b.sem, 16)
        ev = ("d_" + b.name, b.sem, 16 * b.dcnt)
        for x in rd:
            x.rds[ev[0]] = ev
        for x in wr:
            x.lw = ev
            x.rds = {}

    def barrier(self, engines=("pe", "dve", "act", "pool", "sp")):
        evs = []
        for E in self.eng.values():
            if E.cnt:
                evs.append((E.name, E.sem, E.cnt))
        for b in self.bufs:
            if b.sem is not None and b.dcnt:
                evs.append(("d_" + b.name, b.sem, 16 * b.dcnt))
        for en in engines:
            E = self.eng[en]
            for (k, sem, v) in evs:
                if k == E.name or E.seen.get(k, 0) >= v:
                    continue
                E.e.wait_ge(sem, v)
                E.seen[k] = v

    def finish(self):
        self.barrier()


class Rot:
    def __init__(self, bufs):
        self.bufs, self.i = bufs, 0

    def get(self):
        b = self.bufs[self.i % len(self.bufs)]
        self.i += 1
        return b


def rot_sbuf(p, name, n, shape, dt):
    return Rot([p.sbuf(name, shape, dt) for _ in range(n)])


def rot_psum(p, name, n, shape, dt=F32):
    return Rot([p.psum(name, shape, dt) for _ in range(n)])


def load_small(p, name, ap, shape, dt=F32, q="sp"):
    b = p.sbuf(name, shape, dt)
    p.dma(q, b[:], ap, wr=[b])
    return b


def load_w(p, wb, w_ap, kch, n, stg):
    wv = w_ap.rearrange("(c p) n -> p c n", p=128)
    step = 2048
    for c in range(kch):
        for n0 in range(0, n, step):
            n1 = min(n, n0 + step)
            s = stg.get()
            p.dma("sp", s[:, 0:n1 - n0], wv[:, c, n0:n1], wr=[s])
            p.op("pool", lambda e: e.tensor_copy(out=wb[:, c, n0:n1], in_=s[:, 0:n1 - n0]), rd=[s], wr=[wb])
    return wb


def rmsnorm_fm(p, xs, N, gs, ones, sq, hs, rstd, ps):
    p.op("act", lambda e: e.activation(out=sq[:, :, 0:N], in_=xs[:, :, 0:N], func=AF.Square), rd=[xs], wr=[sq])
    for c in range(8):
        p.op("pe", lambda e: e.matmul(ps[:, 0:N], lhsT=ones[:], rhs=sq[:, c, 0:N], start=(c == 0), stop=(c == 7)),
             rd=[ones, sq], wr=[ps])
    p.op("act", lambda e: e.activation(out=rstd[:, 0:N], in_=ps[:, 0:N], func=AF.Sqrt, scale=1.0 / D, bias=EPS),
         rd=[ps], wr=[rstd])
    p.op("dve", lambda e: e.reciprocal(out=rstd[:, 0:N], in_=rstd[:, 0:N]), rd=[rstd], wr=[rstd])
    for c in range(8):
        p.op("dve", lambda e: e.scalar_tensor_tensor(out=hs[:, c, 0:N], in0=xs[:, c, 0:N], scalar=gs[:, c:c + 1],
                                                     in1=rstd[:, 0:N], op0=ALU.mult, op1=ALU.mult),
             rd=[xs, gs, rstd], wr=[hs])


def conv_fm(p, ub, N, K, cwb, j, carry, acc, first):
    wk = cwb[:, j, :]
    if first:
        p.op("pool", lambda e: e.memset(ub[:, 0:K - 1], 0.0), wr=[ub])
    else:
        p.op("pool", lambda e: e.tensor_copy(out=ub[:, 0:K - 1], in_=carry[:]), rd=[carry], wr=[ub])
    p.op("pool", lambda e: e.tensor_copy(out=carry[:], in_=ub[:, N:N + K - 1]), rd=[ub], wr=[carry])
    p.op("pool", lambda e: e.tensor_scalar(out=acc[:, 0:N], in0=ub[:, K - 1:K - 1 + N], scalar1=wk[:, K - 1:K],
                                           scalar2=None, op0=ALU.mult), rd=[ub, cwb], wr=[acc])
    for k in range(K - 1):
        p.op("dve", lambda e: e.scalar_tensor_tensor(out=acc[:, 0:N], in0=ub[:, k:k + N], scalar=wk[:, k:k + 1],
                                                     in1=acc[:, 0:N], op0=ALU.mult, op1=ALU.add),
             rd=[ub, acc, cwb], wr=[acc])


def tok_tiles(T, nt):
    tiles = []
    t = 0
    while t < T:
        n = min(nt, T - t)
        tiles.append((t, n))
        t += n
    return tiles


def pass_proj(p, T, xT, g_ap, w_ap, FO, outT, out_dt, nconv=0, K=0, cw_ap=None, scale_chunks=(), scale=1.0, NT=512):
    with p.scope():
        W = p.sbuf("W", [128, 8, FO], BF16)
        with p.scope():
            load_w(p, W, w_ap, 8, FO, rot_sbuf(p, "stg", 2, [128, 2048], F32))
        gs = load_small(p, "gs", g_ap, [128, 8])
        nch = (FO + 127) // 128
        if nconv:
            cw = load_small(p, "cw", cw_ap, [128, nconv, K])
            carry = [p.sbuf("carry", [128, K - 1], F32) for _ in range(nconv)]
            ubs = rot_sbuf(p, "ub", 2, [128, K - 1 + NT], F32)
            accs = rot_sbuf(p, "acc", 2, [128, NT], F32)
        ones = p.sbuf("ones", [128, 128], BF16)
        p.op("dve", lambda e: e.memset(ones[:], 1.0), wr=[ones])
        xsr = rot_sbuf(p, "xs", 2, [128, 8, NT], F32)
        sq = p.sbuf("sq", [128, 8, NT], BF16)
        hsr = rot_sbuf(p, "hs", 2, [128, 8, NT], BF16)
        rstd = p.sbuf("rstd", [128, NT], F32)
        obr = rot_sbuf(p, "ob", 3, [128, NT], out_dt)
        ps_ss = p.psum("ps_ss", [128, 512])
        psr = rot_psum(p, "ps", 3, [128, 512])
        xv = xT.rearrange("(c p) t -> p c t", p=128)
        for ti, (t0, N) in enumerate(tok_tiles(T, NT)):
            xs = xsr.get()
            p.dma("sp", xs[:, :, 0:N], xv[:, :, t0:t0 + N], wr=[xs])
            hs = hsr.get()
            rmsnorm_fm(p, xs, N, gs, ones, sq, hs, rstd, ps_ss)
            for j in range(nch):
                M = min(128, FO - j * 128)
                ps = psr.get()
                for c in range(8):
                    p.op("pe", lambda e: e.matmul(ps[0:M, 0:N], lhsT=W[:, c, j * 128:j * 128 + M], rhs=hs[:, c, 0:N],
                                                  start=(c == 0), stop=(c == 7)), rd=[W, hs], wr=[ps])
                ob = obr.get()
                if j < nconv:
                    ub = ubs.get()
                    acc = accs.get()
                    p.op("act", lambda e: e.activation(out=ub[:, K - 1:K - 1 + N], in_=ps[:, 0:N], func=AF.Copy),
                         rd=[ps], wr=[ub])
                    conv_fm(p, ub, N, K, cw, j, carry[j], acc, ti == 0)
                    p.op("act", lambda e: e.activation(out=ob[:, 0:N], in_=acc[:, 0:N], func=AF.Silu),
                         rd=[acc], wr=[ob])
                else:
                    sc = scale if j in scale_chunks else 1.0
                    p.op("act", lambda e: e.activation(out=ob[0:M, 0:N], in_=ps[0:M, 0:N], func=AF.Copy, scale=sc),
                         rd=[ps], wr=[ob])
                p.dma("pool", outT[j * 128:j * 128 + M, t0:t0 + N], ob[0:M, 0:N], rd=[ob])


def pass_oproj(p, T, xT, oT, o_dt, w_ap, xoutT, NT=512):
    with p.scope():
        W = p.sbuf("Wo", [128, 8, D], BF16)
        with p.scope():
            load_w(p, W, w_ap, 8, D, rot_sbuf(p, "stg", 2, [128, 2048], F32))
        xsr = rot_sbuf(p, "xs", 2, [128, 8, NT], F32)
        osr = rot_sbuf(p, "os", 2, [128, 8, NT], o_dt)
        if o_dt != BF16:
            obr = rot_sbuf(p, "obf", 2, [128, 8, NT], BF16)
        psr = rot_psum(p, "ps", 3, [128, 512])
        xv = xT.rearrange("(c p) t -> p c t", p=128)
        ov = oT.rearrange("(c p) t -> p c t", p=128)
        xov = xoutT.rearrange("(c p) t -> p c t", p=128)
        for ti, (t0, N) in enumerate(tok_tiles(T, NT)):
            xs = xsr.get()
            p.dma("sp", xs[:, :, 0:N], xv[:, :, t0:t0 + N], wr=[xs])
            os_ = osr.get()
            p.dma("sp", os_[:, :, 0:N], ov[:, :, t0:t0 + N], wr=[os_])
            if o_dt != BF16:
                ob = obr.get()
                p.op("pool", lambda e: e.tensor_copy(out=ob[:, :, 0:N], in_=os_[:, :, 0:N]), rd=[os_], wr=[ob])
                os_ = ob
            for fo in range(8):
                ps = psr.get()
                for c in range(8):
                    p.op("pe", lambda e: e.matmul(ps[:, 0:N], lhsT=W[:, c, fo * 128:(fo + 1) * 128], rhs=os_[:, c, 0:N],
                                                  start=(c == 0), stop=(c == 7)), rd=[W, os_], wr=[ps])
                p.op("dve", lambda e: e.tensor_tensor(out=xs[:, fo, 0:N], in0=xs[:, fo, 0:N], in1=ps[:, 0:N], op=ALU.add),
                     rd=[xs, ps], wr=[xs])
            p.dma("pool", xov[:, :, t0:t0 + N], xs[:, :, 0:N], rd=[xs])


def pass_ffn(p, T, xT, g_ap, wup_ap, cw_ap, wdn_ap, xoutT, final_g_ap=None, NT=256):
    NJ = FFN // 128
    with p.scope():
        Wu = p.sbuf("Wu", [128, 8, 2 * FFN], BF16)
        Wd = p.sbuf("Wd", [128, NJ, D], BF16)
        with p.scope():
            stg = rot_sbuf(p, "stg", 2, [128, 2048], F32)
            load_w(p, Wu, wup_ap, 8, 2 * FFN, stg)
            load_w(p, Wd, wdn_ap, NJ, D, stg)
        gs = load_small(p, "gs", g_ap, [128, 8])
        cw = load_small(p, "cw", cw_ap, [128, 2 * NJ, 3])
        if final_g_ap is not None:
            gf = load_small(p, "gf", final_g_ap, [128, 8])
            onesf = p.sbuf("onesf", [128, 128], F32)
            p.op("dve", lambda e: e.memset(onesf[:], 1.0), wr=[onesf])
            sqf = p.sbuf("sqf", [128, 8, NT], F32)
        carry = [p.sbuf("carry", [128, 2], F32) for _ in range(2 * NJ)]
        ones = p.sbuf("ones", [128, 128], BF16)
        p.op("dve", lambda e: e.memset(ones[:], 1.0), wr=[ones])
        xsr = rot_sbuf(p, "xs", 2, [128, 8, NT], F32)
        sq = p.sbuf("sq", [128, 8, NT], BF16)
        hs = p.sbuf("hs", [128, 8, NT], BF16)
        rstd = p.sbuf("rstd", [128, NT], F32)
        act = p.sbuf("actT", [128, NJ, NT], BF16)
        ubr = rot_sbuf(p, "ub", 4, [128, 2 + NT], F32)
        acr = rot_sbuf(p, "acc", 4, [128, NT], F32)
        sgr = rot_sbuf(p, "sg", 2, [128, NT], F32)
        ps_ss = p.psum("ps_ss", [128, 512])
        psr = rot_psum(p, "ps", 4, [128, 512])
        pso = rot_psum(p, "pso", 2, [128, 512])
        xv = xT.rearrange("(c p) t -> p c t", p=128)
        xov = xoutT.rearrange("(c p) t -> p c t", p=128)
        for ti, (t0, N) in enumerate(tok_tiles(T, NT)):
            xs = xsr.get()
            p.dma("sp", xs[:, :, 0:N], xv[:, :, t0:t0 + N], wr=[xs])
            rmsnorm_fm(p, xs, N, gs, ones, sq, hs, rstd, ps_ss)
            for j in range(NJ):
                accs = []
                for half in range(2):
                    col = half * FFN + j * 128
                    ps = psr.get()
                    for c in range(8):
                        p.op("pe", lambda e: e.matmul(ps[:, 0:N], lhsT=Wu[:, c, col:col + 128], rhs=hs[:, c, 0:N],
                                                      start=(c == 0), stop=(c == 7)), rd=[Wu, hs], wr=[ps])
                    ub = ubr.get()
                    acc = acr.get()
                    p.op("act", lambda e: e.activation(out=ub[:, 2:2 + N], in_=ps[:, 0:N], func=AF.Copy),
                         rd=[ps], wr=[ub])
                    conv_fm(p, ub, N, 3, cw, half * NJ + j, carry[half * NJ + j], acc, ti == 0)
                    accs.append(acc)
                sg = sgr.get()
                p.op("act", lambda e: e.activation(out=sg[:, 0:N], in_=accs[0][:, 0:N], func=AF.Silu),
                     rd=[accs[0]], wr=[sg])
                p.op("dve", lambda e: e.tensor_tensor(out=act[:, j, 0:N], in0=sg[:, 0:N], in1=accs[1][:, 0:N],
                                                      op=ALU.mult), rd=[sg, accs[1]], wr=[act])
            for fo in range(8):
                ps = pso.get()
                for j in range(NJ):
                    p.op("pe", lambda e: e.matmul(ps[:, 0:N], lhsT=Wd[:, j, fo * 128:(fo + 1) * 128], rhs=act[:, j, 0:N],
                                                  start=(j == 0), stop=(j == NJ - 1)), rd=[Wd, act], wr=[ps])
                p.op("dve", lambda e: e.tensor_tensor(out=xs[:, fo, 0:N], in0=xs[:, fo, 0:N], in1=ps[:, 0:N], op=ALU.add),
                     rd=[xs, ps], wr=[xs])
            if final_g_ap is not None:
                p.op("act", lambda e: e.activation(out=sqf[:, :, 0:N], in_=xs[:, :, 0:N], func=AF.Square),
                     rd=[xs], wr=[sqf])
                for c in range(8):
                    p.op("pe", lambda e: e.matmul(ps_ss[:, 0:N], lhsT=onesf[:], rhs=sqf[:, c, 0:N], start=(c == 0),
                                                  stop=(c == 7)), rd=[onesf, sqf], wr=[ps_ss])
                p.op("act", lambda e: e.activation(out=rstd[:, 0:N], in_=ps_ss[:, 0:N], func=AF.Sqrt, scale=1.0 / D,
                                                   bias=EPS), rd=[ps_ss], wr=[rstd])
                p.op("dve", lambda e: e.reciprocal(out=rstd[:, 0:N], in_=rstd[:, 0:N]), rd=[rstd], wr=[rstd])
                for c in range(8):
                    p.op("dve", lambda e: e.scalar_tensor_tensor(out=xs[:, c, 0:N], in0=xs[:, c, 0:N],
                                                                 scalar=gf[:, c:c + 1], in1=rstd[:, 0:N],
                                                                 op0=ALU.mult, op1=ALU.mult),
                         rd=[xs, gf, rstd], wr=[xs])
            p.dma("pool", xov[:, :, t0:t0 + N], xs[:, :, 0:N], rd=[xs])


def pass_sconv(p, T, xT, g_ap, win_ap, cw_ap, wout_ap, xoutT, NT=512):
    with p.scope():
        Wi = p.sbuf("Wi", [128, 8, 3 * D], BF16)
        Wo = p.sbuf("Wo", [128, 8, D], BF16)
        with p.scope():
            stg = rot_sbuf(p, "stg", 2, [128, 2048], F32)
            load_w(p, Wi, win_ap, 8, 3 * D, stg)
            load_w(p, Wo, wout_ap, 8, D, stg)
        gs = load_small(p, "gs", g_ap, [128, 8])
        cw = load_small(p, "cw", cw_ap, [128, 8, 3])
        carry = [p.sbuf("carry", [128, 2], F32) for _ in range(8)]
        ones = p.sbuf("ones", [128, 128], BF16)
        p.op("dve", lambda e: e.memset(ones[:], 1.0), wr=[ones])
        xsr = rot_sbuf(p, "xs", 2, [128, 8, NT], F32)
        sq = p.sbuf("sq", [128, 8, NT], BF16)
        hs = p.sbuf("hs", [128, 8, NT], BF16)
        rstd = p.sbuf("rstd", [128, NT], F32)
        ys = p.sbuf("ys", [128, 8, NT], BF16)
        ubr = rot_sbuf(p, "ub", 2, [128, 2 + NT], F32)
        acr = rot_sbuf(p, "acc", 2, [128, NT], F32)
        tmr = rot_sbuf(p, "tm", 2, [128, NT], F32)
        ps_ss = p.psum("ps_ss", [128, 512])
        psr = rot_psum(p, "ps", 6, [128, 512])
        xv = xT.rearrange("(c p) t -> p c t", p=128)
        xov = xoutT.rearrange("(c p) t -> p c t", p=128)
        for ti, (t0, N) in enumerate(tok_tiles(T, NT)):
            xs = xsr.get()
            p.dma("sp", xs[:, :, 0:N], xv[:, :, t0:t0 + N], wr=[xs])
            rmsnorm_fm(p, xs, N, gs, ones, sq, hs, rstd, ps_ss)
            for j in range(8):
                pss = []
                for part in range(3):
                    col = part * D + j * 128
                    ps = psr.get()
                    for c in range(8):
                        p.op("pe", lambda e: e.matmul(ps[:, 0:N], lhsT=Wi[:, c, col:col + 128], rhs=hs[:, c, 0:N],
                                                      start=(c == 0), stop=(c == 7)), rd=[Wi, hs], wr=[ps])
                    pss.append(ps)
                ub = ubr.get()
                acc = acr.get()
                tm = tmr.get()
                p.op("act", lambda e: e.activation(out=tm[:, 0:N], in_=pss[1][:, 0:N], func=AF.Copy),
                     rd=[pss[1]], wr=[tm])
                p.op("dve", lambda e: e.tensor_tensor(out=ub[:, 2:2 + N], in0=tm[:, 0:N], in1=pss[2][:, 0:N],
                                                      op=ALU.mult), rd=[tm, pss[2]], wr=[ub])
                conv_fm(p, ub, N, 3, cw, j, carry[j], acc, ti == 0)
                p.op("dve", lambda e: e.tensor_tensor(out=ys[:, j, 0:N], in0=acc[:, 0:N], in1=pss[0][:, 0:N],
                                                      op=ALU.mult), rd=[acc, pss[0]], wr=[ys])
            for fo in range(8):
                ps = psr.get()
                for c in range(8):
                    p.op("pe", lambda e: e.matmul(ps[:, 0:N], lhsT=Wo[:, c, fo * 128:(fo + 1) * 128], rhs=ys[:, c, 0:N],
                                                  start=(c == 0), stop=(c == 7)), rd=[Wo, ys], wr=[ps])
                p.op("dve", lambda e: e.tensor_tensor(out=xs[:, fo, 0:N], in0=xs[:, fo, 0:N], in1=ps[:, 0:N], op=ALU.add),
                     rd=[xs, ps], wr=[xs])
            p.dma("pool", xov[:, :, t0:t0 + N], xs[:, :, 0:N], rd=[xs])


def moba_attention(p, S, nb, qT, kT, v, oT, ident_ap, esel_ap, cmask_ap, ktab_ap, qrows_ap):
    NB = S // 256
    NQT = S // 512
    NKC = S // 128
    with p.scope():
        ident = load_small(p, "ident", ident_ap, [128, 128], BF16)
        esel = load_small(p, "esel", esel_ap, [128, 64, 128], BF16)
        cmask = load_small(p, "cmask", cmask_ap, [128, 4, 512], BF16)
        ktab = load_small(p, "ktab", ktab_ap, [128, 128], F32)
        onesf = p.sbuf("onesf", [128, 128], F32)
        p.op("dve", lambda e: e.memset(onesf[:], 1.0), wr=[onesf])
        kTs = p.sbuf("kTs", [128, S], BF16)
        qTs = p.sbuf("qTs", [128, S], BF16)
        vs = p.sbuf("vs", [128, NKC, 128], BF16)
        kmf = p.sbuf("kmf", [128, 64], F32)
        kmb = p.sbuf("kmb", [128, 64], BF16)
        bTr = rot_sbuf(p, "biasT", 2, [128, 512], BF16)
        for b_ in bTr.bufs:
            p.op("pool", lambda e: e.memset(b_[:], 0.0), wr=[b_])
            p.dma("sp", b_[64:66, :], qrows_ap, wr=[b_])
        gsbr = rot_sbuf(p, "gsb", 2, [128, 64], F32)
        t8r = rot_sbuf(p, "top8", 2, [128, 8], F32)
        mskr = rot_sbuf(p, "msk", 2, [128, 64], BF16)
        PTr = rot_sbuf(p, "PT", 3, [128, 512], BF16)
        raccr = rot_sbuf(p, "racc", 2, [128, 512], F32)
        rinv = p.sbuf("rinv", [128, 512], F32)
        otr = rot_sbuf(p, "ot", 2, [128, 512], BF16)
        psSr = rot_psum(p, "psS", 3, [128, 512])
        psOr = rot_psum(p, "psO", 2, [128, 512])
        psg = p.psum("psg", [128, 64])
        pst = p.psum("pst", [64, 128], BF16)
        psr_ = p.psum("psr", [128, 512])
        for b in range(nb):
            p.dma("sp", kTs[:], kT[b], wr=[kTs])
            p.dma("sp", qTs[:], qT[b], wr=[qTs])
            p.dma("sp", vs[:], v[b].rearrange("(c p) d -> p c d", p=128), wr=[vs])
            p.op("pool", lambda e: e.memset(kmf[:], 0.0), wr=[kmf])
            p.op("dve", lambda e: e.tensor_reduce(out=kmf[:, 0:NB], in_=kTs[:].rearrange("p (n k) -> p n k", k=256),
                                                  axis=AX.X, op=ALU.add), rd=[kTs], wr=[kmf])
            p.op("dve", lambda e: e.tensor_copy(out=kmb[:], in_=kmf[:]), rd=[kmf], wr=[kmb])
            for qt in range(NQT):
                t0 = qt * 512
                bT = bTr.get()
                for s in range(4):
                    own = 2 * qt + s // 2
                    msk = mskr.get()
                    if own == 0:
                        p.op("pool", lambda e: e.memset(msk[:], 0.0), wr=[msk])
                    else:
                        p.op("pe", lambda e: e.matmul(psg[:], lhsT=qTs[:, t0 + 128 * s:t0 + 128 * s + 128], rhs=kmb[:],
                                                      start=True, stop=True), rd=[qTs, kmb], wr=[psg])
                        gsb = gsbr.get()
                        p.op("pool", lambda e: e.memset(gsb[:], -1e30), wr=[gsb])
                        p.op("act", lambda e: e.activation(out=gsb[:, 0:own], in_=psg[:, 0:own], func=AF.Copy),
                             rd=[psg], wr=[gsb])
                        t8 = t8r.get()
                        p.op("dve", lambda e: e.max(out=t8[:], in_=gsb[:]), rd=[gsb], wr=[t8])
                        p.op("dve", lambda e: e.tensor_scalar(out=msk[:], in0=gsb[:], scalar1=t8[:, 2:3], scalar2=-1.0,
                                                              op0=ALU.is_ge, op1=ALU.add), rd=[gsb, t8], wr=[msk])
                        hi = min(64, own + 2)
                        p.op("pool", lambda e: e.memset(msk[:, own:hi], 0.0), wr=[msk])
                    p.op("pe", lambda e: e.transpose(out=pst[:], in_=msk[:], identity=ident[:]), rd=[msk, ident], wr=[pst])
                    p.op("act", lambda e: e.activation(out=bT[0:64, 128 * s:128 * s + 128], in_=pst[:], func=AF.Copy),
                         rd=[pst], wr=[bT])
                psO = psOr.get()
                racc = raccr.get()
                nkc = 4 * qt + 4

                def emit_S(kc):
                    n = kc // 2
                    d = kc - 4 * qt
                    psS = psSr.get()
                    p.op("pe", lambda e: e.matmul(psS[:], lhsT=kTs[:, kc * 128:kc * 128 + 128], rhs=qTs[:, t0:t0 + 512],
                                                  start=True, stop=False), rd=[kTs, qTs], wr=[psS])
                    p.op("pe", lambda e: e.matmul(psS[:], lhsT=esel[:, n, :], rhs=bT[:], start=False, stop=(d < 0)),
                         rd=[esel, bT], wr=[psS])
                    if d >= 0:
                        p.op("pe", lambda e: e.matmul(psS[:], lhsT=ident[:], rhs=cmask[:, d, :], start=False, stop=True),
                             rd=[ident, cmask], wr=[psS])
                    return psS

                psS_next = emit_S(0)
                for kc in range(nkc):
                    psS = psS_next
                    if kc + 1 < nkc:
                        psS_next = emit_S(kc + 1)
                    PT = PTr.get()
                    m = 4 * qt + 3 - kc
                    p.op("act", lambda e: e.activation(out=PT[:], in_=psS[:], func=AF.Exp, bias=ktab[:, m:m + 1], scale=1.0),
                         rd=[psS, ktab], wr=[PT])
                    p.op("pe", lambda e: e.matmul(psO[:], lhsT=vs[:, kc, :], rhs=PT[:], start=(kc == 0),
                                                  stop=(kc == nkc - 1)), rd=[vs, PT], wr=[psO])
                    if kc == 0:
                        p.op("dve", lambda e: e.tensor_copy(out=racc[:], in_=PT[:]), rd=[PT], wr=[racc])
                    else:
                        p.op("dve", lambda e: e.tensor_tensor(out=racc[:], in0=racc[:], in1=PT[:], op=ALU.add),
                             rd=[racc, PT], wr=[racc])
                p.op("pe", lambda e: e.matmul(psr_[:], lhsT=onesf[:], rhs=racc[:], start=True, stop=True),
                     rd=[onesf, racc], wr=[psr_])
                p.op("dve", lambda e: e.reciprocal(out=rinv[:], in_=psr_[:]), rd=[psr_], wr=[rinv])
                ot = otr.get()
                p.op("dve", lambda e: e.tensor_tensor(out=ot[:], in0=psO[:], in1=rinv[:], op=ALU.mult),
                     rd=[psO, rinv], wr=[ot])
                p.dma("pool", oT[b][:, t0:t0 + 512], ot[:], rd=[ot])


def new_nc():
    return bass.Bass("TRN2", target_bir_lowering=False)


def din(nc, name, shape, dt=F32):
    return nc.dram_tensor(name, list(shape), dt, kind="ExternalInput").ap()


def dout(nc, name, shape, dt=F32):
    return nc.dram_tensor(name, list(shape), dt, kind="ExternalOutput").ap()


def dtmp(nc, name, shape, dt=F32):
    return nc.dram_tensor(name, list(shape), dt, kind="Internal").ap()


def ffn_inputs(nc, tag):
    return dict(g=din(nc, tag + "_g", [128, 8]), wup=din(nc, tag + "_wup", [D, 2 * FFN]),
                cw=din(nc, tag + "_cw", [128, 2 * (FFN // 128), 3]), wdn=din(nc, tag + "_wdn", [FFN, D]))


def build_A(T):
    nc = new_nc()
    xT = din(nc, "xT", [D, T])
    g = din(nc, "g", [128, 8])
    w = din(nc, "w", [D, 3 * D])
    o = dout(nc, "qkvT", [3 * D, T], BF16)
    with ExitStack() as es:
        p = Prog(nc, es)
        pass_proj(p, T, xT, g, w, 3 * D, o, BF16, scale_chunks=range(8), scale=HD ** -0.5)
        p.finish()
    return nc


def build_B(S):
    nc = new_nc()
    qT = din(nc, "qT", [2, 128, S], BF16)
    kT = din(nc, "kT", [2, 128, S], BF16)
    v = din(nc, "v", [2, S, 128], BF16)
    ident = din(nc, "ident", [128, 128], BF16)
    esel = din(nc, "esel", [128, 64, 128], BF16)
    cmask = din(nc, "cmask", [128, 4, 512], BF16)
    ktab = din(nc, "ktab", [128, 128])
    qrows = din(nc, "qrows", [2, 512], BF16)
    oT = dout(nc, "oT", [2, 128, S], BF16)
    with ExitStack() as es:
        p = Prog(nc, es)
        moba_attention(p, S, 2, qT, kT, v, oT, ident, esel, cmask, ktab, qrows)
        p.finish()
    return nc


def build_G(T, final):
    nc = new_nc()
    xT = din(nc, "xT", [D, T])
    oT = din(nc, "oT", [D, T], BF16)
    wo = din(nc, "wo", [D, D])
    f = ffn_inputs(nc, "f")
    gf = din(nc, "gf", [128, 8]) if final else None
    xm = dtmp(nc, "xm", [D, T])
    xo = dout(nc, "xoT", [D, T])
    with ExitStack() as es:
        p = Prog(nc, es)
        pass_oproj(p, T, xT, oT, BF16, wo, xm)
        pass_ffn(p, T, xm, f["g"], f["wup"], f["cw"], f["wdn"], xo, final_g_ap=gf)
        p.finish()
    return nc


def run(nc, in_maps):
    res = run_bass_kernel_spmd(nc, in_maps, core_ids=list(range(NCORE)))
    return res.results


def shard_T(full, S):
    TQ = S // 4
    outs = []
    for c in range(NCORE):
        b, qd = divmod(c, 4)
        lo = qd * TQ - HALO
        if lo < 0:
            blk = np.concatenate([np.zeros((HALO, full.shape[2]), full.dtype), full[b, 0:TQ]], axis=0)
        else:
            blk = full[b, lo:lo + HALO + TQ]
        outs.append(np.ascontiguousarray(blk.T))
    return outs


def unshard_T(parts, S):
    TQ = S // 4
    F = parts[0].shape[0]
    full = np.empty((2, S, F), parts[0].dtype)
    for c in range(NCORE):
        b, qd = divmod(c, 4)
        full[b, qd * TQ:(qd + 1) * TQ] = parts[c][:, HALO:].T
    return full


def vec128(v):
    return np.ascontiguousarray(v.reshape(-1, 128).T)


def conv128(w):
    K, C = w.shape
    return np.ascontiguousarray(w.T.reshape(C // 128, 128, K).transpose(1, 0, 2))


def ffn_maps(tag, g, wup, cw, wdn):
    return {tag + "_g": vec128(g), tag + "_wup": wup, tag + "_cw": conv128(cw), tag + "_wdn": wdn}


def moba_consts(h):
    slope = 2.0 ** (-(h + 1))
    ident = np.eye(128, dtype=np.float32).astype(NPBF)
    esel = np.zeros((128, 64, 128), np.float32)
    for n in range(64):
        esel[n, n, :] = BIG
    esel[64:66, :, :] = 1.0
    j = np.arange(128)[:, None]
    i = np.arange(512)[None, :]
    cmask = np.zeros((128, 4, 512), np.float32)
    for d in range(4):
        cmask[:, d, :] = np.where(128 * d + j <= i, 0.0, -BIG)
    m = np.arange(128)[None, :]
    ktab = (slope * (j - 127 - 128 * m)).astype(np.float32)
    r = 511 - np.arange(512)
    qrows = np.stack([slope * (r // 4 * 4), slope * (r % 4)]).astype(np.float32)
    return dict(ident=ident, esel=esel.astype(NPBF), cmask=cmask.astype(NPBF), ktab=ktab, qrows=qrows.astype(NPBF))


_CACHE = {}


def cached(key, fn):
    if key not in _CACHE:
        _CACHE[key] = fn()
    return _CACHE[key]


def moba_layer_mix(x_full, S, g, wqkv):
    T = HALO + S // 4
    ncA = cached(("A", T), lambda: build_A(T))
    xs = shard_T(x_full, S)
    resA = run(ncA, [{"xT": xs[c], "g": vec128(g), "w": wqkv} for c in range(NCORE)])
    qkv = unshard_T([r["qkvT"] for r in resA], S)
    ncB = cached(("B", S), lambda: build_B(S))
    maps = []
    for h in range(NH):
        m = dict(qT=np.ascontiguousarray(qkv[:, :, h * 128:(h + 1) * 128].transpose(0, 2, 1)),
                 kT=np.ascontiguousarray(qkv[:, :, D + h * 128:D + (h + 1) * 128].transpose(0, 2, 1)),
                 v=np.ascontiguousarray(qkv[:, :, 2 * D + h * 128:2 * D + (h + 1) * 128]))
        m.update(moba_consts(h))
        maps.append(m)
    resB = run(ncB, maps)
    o_full = np.concatenate([r["oT"].transpose(0, 2, 1) for r in resB], axis=2)
    return xs, shard_T(o_full, S)


def gdn_scan(p, S, q, k, v, z, atab, btab, alog_ap, dtb_ap, normw_ap, U_ap, SL_ap, SU_ap, UI_ap, id64_ap, y):
    NCH = S // 64
    G = 8
    NG = NCH // G
    with p.scope():
        U = load_small(p, "U", U_ap, [64, 64])
        SL = load_small(p, "SL", SL_ap, [64, 64])
        SU = load_small(p, "SU", SU_ap, [64, 64])
        UI = load_small(p, "UI", UI_ap, [64, 64])
        id64 = load_small(p, "id64", id64_ap, [64, 64])
        normw = load_small(p, "normw", normw_ap, [64, 128])
        alog = load_small(p, "alog", alog_ap, [128, 1])
        dtb = load_small(p, "dtb", dtb_ap, [128, 1])
        ones64 = p.sbuf("ones64", [64, 128], F32)
        p.op("dve", lambda e: e.memset(ones64[:], 1.0), wr=[ones64])
        nega = p.sbuf("nega", [128, 1], F32)
        p.op("act", lambda e: e.activation(out=nega[:], in_=alog[:], func=AF.Exp), rd=[alog], wr=[nega])
        p.op("dve", lambda e: e.tensor_scalar(out=nega[:], in0=nega[:], scalar1=-1.0, scalar2=None, op0=ALU.mult),
             rd=[nega], wr=[nega])
        gtab, betab = [], []
        for b in range(2):
            at = load_small(p, "at", atab[b], [64, NCH])
            bt = load_small(p, "bt", btab[b], [64, NCH])
            xx = p.sbuf("xx", [64, NCH], F32)
            ax = p.sbuf("ax", [64, NCH], F32)
            gt = p.sbuf("gt", [64, NCH], F32)
            be = p.sbuf("be", [64, NCH], F32)
            p.op("dve", lambda e: e.tensor_scalar(out=xx[:], in0=at[:], scalar1=dtb[0:64, 0:1], scalar2=None, op0=ALU.add),
                 rd=[at, dtb], wr=[xx])
            p.op("dve", lambda e: e.tensor_scalar(out=ax[:], in0=xx[:], scalar1=-1.0, scalar2=None, op0=ALU.mult),
                 rd=[xx], wr=[ax])
            p.op("dve", lambda e: e.tensor_tensor(out=ax[:], in0=ax[:], in1=xx[:], op=ALU.min), rd=[ax, xx], wr=[ax])
            p.op("act", lambda e: e.activation(out=ax[:], in_=ax[:], func=AF.Exp), rd=[ax], wr=[ax])
            p.op("act", lambda e: e.activation(out=ax[:], in_=ax[:], func=AF.Ln, bias=1.0, scale=1.0), rd=[ax], wr=[ax])
            p.op("dve", lambda e: e.tensor_scalar(out=xx[:], in0=xx[:], scalar1=0.0, scalar2=None, op0=ALU.max),
                 rd=[xx], wr=[xx])
            p.op("dve", lambda e: e.tensor_tensor(out=xx[:], in0=xx[:], in1=ax[:], op=ALU.add), rd=[xx, ax], wr=[xx])
            p.op("dve", lambda e: e.tensor_scalar(out=gt[:], in0=xx[:], scalar1=nega[0:64, 0:1], scalar2=None, op0=ALU.mult),
                 rd=[xx, nega], wr=[gt])
            p.op("act", lambda e: e.activation(out=be[:], in_=bt[:], func=AF.Exp, scale=-1.0), rd=[bt], wr=[be])
            p.op("dve", lambda e: e.tensor_scalar(out=be[:], in0=be[:], scalar1=1.0, scalar2=None, op0=ALU.add),
                 rd=[be], wr=[be])
            p.op("dve", lambda e: e.reciprocal(out=be[:], in_=be[:]), rd=[be], wr=[be])
            gtab.append(gt)
            betab.append(be)

        rots = {}

        def Tm(name, shape, n=2):
            if name not in rots:
                rots[name] = rot_sbuf(p, name, n, shape, F32)
            return rots[name].get()

        PS = rot_psum(p, "ps", 7, [128, 512])
        Sst = [[p.sbuf("S", [128, 128], F32) for _ in range(2)] for _ in range(2)]
        for b in range(2):
            p.op("pool", lambda e: e.memset(Sst[b][0][:], 0.0), wr=[Sst[b][0]])
        gq = [rot_sbuf(p, "gq", 2, [64, G, 128], F32) for _ in range(2)]
        gk = [rot_sbuf(p, "gk", 2, [64, G, 128], F32) for _ in range(2)]
        gv = [rot_sbuf(p, "gv", 2, [64, G, 128], F32) for _ in range(2)]
        gz = [rot_sbuf(p, "gz", 2, [64, G, 128], F32) for _ in range(2)]
        gy = [rot_sbuf(p, "gy", 2, [64, G, 128], F32) for _ in range(2)]

        def mm(out_ap, ps, lhsT, rhs, rd, start=True, stop=True):
            p.op("pe", lambda e: e.matmul(out_ap, lhsT=lhsT, rhs=rhs, start=start, stop=stop), rd=rd, wr=[ps])

        def evac(eng, name, shape, ps, ps_ap):
            t = Tm(name, shape)
            if eng == "act":
                p.op("act", lambda e: e.activation(out=t[:], in_=ps_ap, func=AF.Copy), rd=[ps], wr=[t])
            else:
                p.op("dve", lambda e: e.tensor_copy(out=t[:], in_=ps_ap), rd=[ps], wr=[t])
            return t

        def tscal(eng, name, shape, src, src_ap, sc, sc_ap, s2=None):
            t = Tm(name, shape)
            if s2 is None:
                p.op(eng, lambda e: e.tensor_scalar(out=t[:], in0=src_ap, scalar1=sc_ap, scalar2=None, op0=ALU.mult),
                     rd=[src, sc], wr=[t])
            else:
                p.op(eng, lambda e: e.tensor_scalar(out=t[:], in0=src_ap, scalar1=sc_ap, scalar2=s2, op0=ALU.mult,
                                                    op1=ALU.mult), rd=[src, sc], wr=[t])
            return t

        def rnorm(src, src_ap, scale_in):
            junk = Tm("junk", [64, 128])
            ss = Tm("ss", [64, 1], 4)
            p.op("dve", lambda e: e.scalar_tensor_tensor(out=junk[:], in0=src_ap, scalar=1.0, in1=src_ap, op0=ALU.mult,
                                                         op1=ALU.mult, accum_out=ss[:]), rd=[src], wr=[junk, ss])
            lg = Tm("lg", [64, 1], 4)
            p.op("act", lambda e: e.activation(out=lg[:], in_=ss[:], func=AF.Ln, bias=EPS, scale=scale_in), rd=[ss], wr=[lg])
            r = Tm("rr", [64, 1], 4)
            p.op("act", lambda e: e.activation(out=r[:], in_=lg[:], func=AF.Exp, scale=-0.5), rd=[lg], wr=[r])
            return r

        def chunk(b, ci, qg, kg, vg, zg, yg, gi):
            gtb, beb = gtab[b], betab[b]
            gcol = gtb[:, ci:ci + 1]
            bcol = beb[:, ci:ci + 1]
            qa, ka, va, za = qg[:, gi, :], kg[:, gi, :], vg[:, gi, :], zg[:, gi, :]
            rq = rnorm(qg, qa, 1.0)
            qn = tscal("dve", "qn", [64, 128], qg, qa, rq, rq[:, 0:1], s2=HD ** -0.5)
            rk = rnorm(kg, ka, 1.0)
            kn = tscal("dve", "kn", [64, 128], kg, ka, rk, rk[:, 0:1])
            kb = tscal("pool", "kb", [64, 128], kn, kn[:], beb, bcol)
            vb = tscal("pool", "vb", [64, 128], vg, va, beb, bcol)
            ps = PS.get()
            mm(ps[0:64, 0:1], ps, U[:], gcol, [U, gtb])
            GC = evac("act", "GC", [64, 1], ps, ps[0:64, 0:1])
            gb = tscal("pool", "gb", [64, 128], ones64, ones64[:], gtb, gcol)
            ps = PS.get()
            mm(ps[:, 0:64], ps, gb[:], U[:], [gb, U])
            GR = evac("act", "GR", [128, 64], ps, ps[:, 0:64])
            egc = Tm("egc", [64, 1], 4)
            p.op("act", lambda e: e.activation(out=egc[:], in_=GC[:], func=AF.Exp), rd=[GC], wr=[egc])
            sdbc = Tm("sdbc", [128, 1], 4)
            p.op("act", lambda e: e.activation(out=sdbc[:], in_=GR[:, 63:64], func=AF.Exp), rd=[GR], wr=[sdbc])
            kdsc = Tm("kdsc", [64, 1], 4)
            p.op("act", lambda e: e.activation(out=kdsc[:], in_=GC[:], func=AF.Exp, scale=-1.0, bias=GR[0:64, 63:64]),
                 rd=[GC, GR], wr=[kdsc])
            dm = Tm("dm", [64, 64])
            p.op("dve", lambda e: e.tensor_scalar(out=dm[:], in0=GR[0:64, :], scalar1=GC[:, 0:1], scalar2=0.0,
                                                  op0=ALU.subtract, op1=ALU.min), rd=[GR, GC], wr=[dm])
            decT = Tm("decT", [64, 64])
            p.op("act", lambda e: e.activation(out=decT[:], in_=dm[:], func=AF.Exp), rd=[dm], wr=[decT])
            dx = Tm("dx", [64, 64])
            p.op("dve", lambda e: e.tensor_scalar(out=dx[:], in0=GR[0:64, :], scalar1=GC[:, 0:1], scalar2=0.0,
                                                  op0=ALU.subtract, op1=ALU.max), rd=[GR, GC], wr=[dx])
            dec = Tm("dec", [64, 64])
            p.op("act", lambda e: e.activation(out=dec[:], in_=dx[:], func=AF.Exp, scale=-1.0), rd=[dx], wr=[dec])
            dSU = Tm("dSU", [64, 64])
            p.op("pool", lambda e: e.tensor_tensor(out=dSU[:], in0=decT[:], in1=SU[:], op=ALU.mult), rd=[decT, SU], wr=[dSU])
            dUI = Tm("dUI", [64, 64])
            p.op("pool", lambda e: e.tensor_tensor(out=dUI[:], in0=decT[:], in1=UI[:], op=ALU.mult), rd=[decT, UI], wr=[dUI])
            dSL = Tm("dSL", [64, 64])
            p.op("pool", lambda e: e.tensor_tensor(out=dSL[:], in0=dec[:], in1=SL[:], op=ALU.mult), rd=[dec, SL], wr=[dSL])
            kbg = tscal("pool", "kbg", [64, 128], kb, kb[:], egc, egc[:, 0:1])
            kd = tscal("pool", "kd", [64, 128], kn, kn[:], kdsc, kdsc[:, 0:1])
            qd = tscal("pool", "qd", [64, 128], qn, qn[:], egc, egc[:, 0:1])
            tr = {}
            for nm, src in (("knT", kn), ("kbT", kb), ("qnT", qn), ("qdT", qd)):
                ps = PS.get()
                p.op("pe", lambda e: e.transpose(out=ps[:, 0:64], in_=src[:], identity=id64[:]), rd=[src, id64], wr=[ps])
                tr[nm] = evac("act" if nm in ("knT", "qnT") else "dve", nm, [128, 64], ps, ps[:, 0:64])
            knT, kbT, qnT, qdT = tr["knT"], tr["kbT"], tr["qnT"], tr["qdT"]

            def masked(nm, lhsT, rhs, msk):
                ps = PS.get()
                mm(ps[0:64, 0:64], ps, lhsT[:], rhs[:], [lhsT, rhs])
                t = Tm(nm, [64, 64])
                p.op("dve", lambda e: e.tensor_tensor(out=t[:], in0=ps[0:64, 0:64], in1=msk[:], op=ALU.mult),
                     rd=[ps, msk], wr=[t])
                return t

            A = masked("A", kbT, knT, dSL)
            B = masked("B", knT, kbT, dSU)
            attnT = masked("attnT", knT, qnT, dUI)
            R = Tm("R", [64, 64], 3)
            p.op("pool", lambda e: e.tensor_tensor(out=R[:], in0=id64[:], in1=B[:], op=ALU.subtract), rd=[id64, B], wr=[R])
            Pp, Qp = A, B
            for kk in range(1, 6):
                ps = PS.get()
                mm(ps[0:64, 0:64], ps, Qp[:], Pp[:], [Qp, Pp])
                Pk = evac("act", "Pk", [64, 64], ps, ps[0:64, 0:64])
                if kk < 5:
                    ps = PS.get()
                    mm(ps[0:64, 0:64], ps, Pp[:], Qp[:], [Pp, Qp])
                    Qk = evac("dve", "Qk", [64, 64], ps, ps[0:64, 0:64])
                ps = PS.get()
                mm(ps[0:64, 0:64], ps, Pk[:], R[:], [Pk, R])
                Rn = Tm("R", [64, 64], 3)
                p.op("dve", lambda e: e.tensor_tensor(out=Rn[:], in0=R[:], in1=ps[0:64, 0:64], op=ALU.add),
                     rd=[R, ps], wr=[Rn])
                R = Rn
                Pp = Pk
                if kk < 5:
                    Qp = Qk
            ps = PS.get()
            mm(ps[0:64, 0:128], ps, R[:], vb[:], [R, vb])
            u = evac("act", "u", [64, 128], ps, ps[0:64, 0:128])
            ps = PS.get()
            mm(ps[:, 0:64], ps, kbg[:], R[:], [kbg, R])
            wT = evac("act", "wT", [128, 64], ps, ps[:, 0:64])
            Sc = Sst[b][ci % 2]
            Sn = Sst[b][(ci + 1) % 2]
            ps = PS.get()
            mm(ps[0:64, 0:128], ps, wT[:], Sc[:], [wT, Sc])
            vnew = Tm("vnew", [64, 128])
            p.op("dve", lambda e: e.tensor_tensor(out=vnew[:], in0=u[:], in1=ps[0:64, 0:128], op=ALU.subtract),
                 rd=[u, ps], wr=[vnew])
            ps3 = PS.get()
            mm(ps3[:, 0:128], ps3, kd[:], vnew[:], [kd, vnew])
            p.op("dve", lambda e: e.scalar_tensor_tensor(out=Sn[:], in0=Sc[:], scalar=sdbc[:, 0:1], in1=ps3[:, 0:128],
                                                         op0=ALU.mult, op1=ALU.add), rd=[Sc, sdbc, ps3], wr=[Sn])
            ps2 = PS.get()
            mm(ps2[0:64, 0:128], ps2, qdT[:], Sc[:], [qdT, Sc], start=True, stop=False)
            mm(ps2[0:64, 0:128], ps2, attnT[:], vnew[:], [attnT, vnew], start=False, stop=True)
            o = evac("act", "o", [64, 128], ps2, ps2[0:64, 0:128])
            ro = rnorm(o, o[:], 1.0 / HD)
            y1 = tscal("pool", "y1", [64, 128], o, o[:], ro, ro[:, 0:1])
            y2 = Tm("y2", [64, 128])
            p.op("pool", lambda e: e.tensor_tensor(out=y2[:], in0=y1[:], in1=normw[:], op=ALU.mult), rd=[y1, normw], wr=[y2])
            ez = Tm("ez", [64, 128])
            p.op("act", lambda e: e.activation(out=ez[:], in_=za, func=AF.Exp, scale=-1.0), rd=[zg], wr=[ez])
            p.op("pool", lambda e: e.tensor_scalar(out=ez[:], in0=ez[:], scalar1=1.0, scalar2=None, op0=ALU.add),
                 rd=[ez], wr=[ez])
            p.op("dve", lambda e: e.reciprocal(out=ez[:], in_=ez[:]), rd=[ez], wr=[ez])
            p.op("pool", lambda e: e.tensor_tensor(out=ez[:], in0=ez[:], in1=za, op=ALU.mult), rd=[ez, zg], wr=[ez])
            p.op("dve", lambda e: e.tensor_tensor(out=yg[:, gi, :], in0=y2[:], in1=ez[:], op=ALU.mult), rd=[y2, ez], wr=[yg])

        for g in range(NG):
            cur = []
            for b in range(2):
                sl = slice(g * G * 64, (g + 1) * G * 64)
                bufs = []
                for pool_, src in ((gq, q), (gk, k), (gv, v), (gz, z)):
                    t = pool_[b].get()
                    p.dma("sp", t[:], src[b][sl, :].rearrange("(g p) d -> p g d", p=64), wr=[t])
                    bufs.append(t)
                bufs.append(gy[b].get())
                cur.append(bufs)
            for gi in range(G):
                for b in range(2):
                    qg, kg, vg, zg, yg = cur[b]
                    chunk(b, g * G + gi, qg, kg, vg, zg, yg, gi)
            for b in range(2):
                sl = slice(g * G * 64, (g + 1) * G * 64)
                yg = cur[b][4]
                p.dma("pool", y[b][sl, :].rearrange("(g p) d -> p g d", p=64), yg[:], rd=[yg])


def build_D(S):
    nc = new_nc()
    q = din(nc, "q", [2, S, 128])
    k = din(nc, "k", [2, S, 128])
    v = din(nc, "v", [2, S, 128])
    z = din(nc, "z", [2, S, 128])
    atab = din(nc, "atab", [2, 64, S // 64])
    btab = din(nc, "btab", [2, 64, S // 64])
    alog = din(nc, "alog", [128, 1])
    dtb = din(nc, "dtb", [128, 1])
    normw = din(nc, "normw", [64, 128])
    U = din(nc, "U", [64, 64])
    SL = din(nc, "SL", [64, 64])
    SU = din(nc, "SU", [64, 64])
    UI = din(nc, "UI", [64, 64])
    id64 = din(nc, "id64", [64, 64])
    y = dout(nc, "y", [2, S, 128])
    with ExitStack() as es:
        p = Prog(nc, es)
        gdn_scan(p, S, q, k, v, z, atab, btab, alog, dtb, normw, U, SL, SU, UI, id64, y)
        p.finish()
    return nc


def build_C(T):
    nc = new_nc()
    xT = din(nc, "xT", [D, T])
    oT = din(nc, "oT", [D, T], BF16)
    wo = din(nc, "wo", [D, D])
    f0 = ffn_inputs(nc, "f0")
    g1 = din(nc, "g1", [128, 8])
    swin = din(nc, "swin", [D, 3 * D])
    scw = din(nc, "scw", [128, 8, 3])
    swout = din(nc, "swout", [D, D])
    f1 = ffn_inputs(nc, "f1")
    g2 = din(nc, "g2", [128, 8])
    gwin = din(nc, "gwin", [D, 4 * D + 16])
    gcw = din(nc, "gcw", [128, 24, 4])
    xm0 = dtmp(nc, "xm0", [D, T])
    x1 = dtmp(nc, "x1", [D, T])
    xm1 = dtmp(nc, "xm1", [D, T])
    x2 = dout(nc, "x2T", [D, T])
    gp = dout(nc, "gpT", [4 * D + 16, T])
    with ExitStack() as es:
        p = Prog(nc, es)
        pass_oproj(p, T, xT, oT, BF16, wo, xm0)
        pass_ffn(p, T, xm0, f0["g"], f0["wup"], f0["cw"], f0["wdn"], x1)
        pass_sconv(p, T, x1, g1, swin, scw, swout, xm1)
        pass_ffn(p, T, xm1, f1["g"], f1["wup"], f1["cw"], f1["wdn"], x2)
        pass_proj(p, T, x2, g2, gwin, 4 * D + 16, gp, F32, nconv=24, K=4, cw_ap=gcw)
        p.finish()
    return nc


def build_E(T):
    nc = new_nc()
    xT = din(nc, "xT", [D, T])
    yT = din(nc, "yT", [D, T])
    wo = din(nc, "wo", [D, D])
    f2 = ffn_inputs(nc, "f2")
    g3 = din(nc, "g3", [128, 8])
    w3 = din(nc, "w3", [D, 3 * D])
    xm = dtmp(nc, "xm", [D, T])
    x3 = dout(nc, "x3T", [D, T])
    qkv = dout(nc, "qkvT", [3 * D, T], BF16)
    with ExitStack() as es:
        p = Prog(nc, es)
        pass_oproj(p, T, xT, yT, F32, wo, xm)
        pass_ffn(p, T, xm, f2["g"], f2["wup"], f2["cw"], f2["wdn"], x3)
        pass_proj(p, T, x3, g3, w3, 3 * D, qkv, BF16, scale_chunks=range(8), scale=HD ** -0.5)
        p.finish()
    return nc


def gdn_consts():
    i = np.arange(64)
    U = (i[:, None] <= i[None, :]).astype(np.float32)
    SL = (i[:, None] > i[None, :]).astype(np.float32)
    SU = (i[None, :] > i[:, None]).astype(np.float32)
    UI = (i[None, :] >= i[:, None]).astype(np.float32)
    return dict(U=U, SL=SL, SU=SU, UI=UI, id64=np.eye(64, dtype=np.float32))


def moba_attn(qkv, S):
    ncB = cached(("B", S), lambda: build_B(S))
    maps = []
    for h in range(NH):
        m = dict(qT=np.ascontiguousarray(qkv[:, :, h * 128:(h + 1) * 128].transpose(0, 2, 1)),
                 kT=np.ascontiguousarray(qkv[:, :, D + h * 128:D + (h + 1) * 128].transpose(0, 2, 1)),
                 v=np.ascontiguousarray(qkv[:, :, 2 * D + h * 128:2 * D + (h + 1) * 128]))
        m.update(moba_consts(h))
        maps.append(m)
    resB = run(ncB, maps)
    return np.concatenate([r["oT"].transpose(0, 2, 1) for r in resB], axis=2)


def forward(inp, S, dbg=None):
    f32 = np.float32
    inp = {k_: np.asarray(v_, dtype=f32) for k_, v_ in inp.items()}
    T = HALO + S // 4
    x = inp["x"]

    def fm(tag, i):
        return ffn_maps(tag, inp["ffn_norm"][i], inp["ffn_w_up"][i], inp["ffn_conv"][i], inp["ffn_w_down"][i])

    ncA = cached(("A", T), lambda: build_A(T))
    xs = shard_T(x, S)
    resA = run(ncA, [{"xT": xs[c], "g": vec128(inp["mix_norm"][0]), "w": inp["moba_w_qkv"][0]} for c in range(NCORE)])
    qkv = unshard_T([r["qkvT"] for r in resA], S)
    os_ = shard_T(moba_attn(qkv, S), S)
    ncC = cached(("C", T), lambda: build_C(T))
    maps = []
    for c in range(NCORE):
        m = {"xT": xs[c], "oT": os_[c], "wo": inp["moba_w_o"][0], "g1": vec128(inp["mix_norm"][1]),
             "swin": inp["sconv_w_in"][0], "scw": conv128(inp["sconv_conv"][0]), "swout": inp["sconv_w_out"][0],
             "g2": vec128(inp["mix_norm"][2]), "gwin": inp["gdn_w_in"][0], "gcw": conv128(inp["gdn_conv"][0])}
        m.update(fm("f0", 0))
        m.update(fm("f1", 1))
        maps.append(m)
    resC = run(ncC, maps)
    x2 = unshard_T([r["x2T"] for r in resC], S)
    gp = unshard_T([r["gpT"] for r in resC], S)
    if dbg is not None:
        dbg["ffn1"] = x2
        dbg["gp"] = gp
    ncD = cached(("D", S), lambda: build_D(S))
    NCH = S // 64
    gc_ = gdn_consts()
    maps = []
    for h in range(NH):
        m = dict(q=np.ascontiguousarray(gp[:, :, h * 128:(h + 1) * 128]),
                 k=np.ascontiguousarray(gp[:, :, D + h * 128:D + (h + 1) * 128]),
                 v=np.ascontiguousarray(gp[:, :, 2 * D + h * 128:2 * D + (h + 1) * 128]),
                 z=np.ascontiguousarray(gp[:, :, 3 * D + h * 128:3 * D + (h + 1) * 128]),
                 btab=np.ascontiguousarray(gp[:, :, 4 * D + h].reshape(2, NCH, 64).transpose(0, 2, 1)),
                 atab=np.ascontiguousarray(gp[:, :, 4 * D + NH + h].reshape(2, NCH, 64).transpose(0, 2, 1)),
                 alog=np.full((128, 1), inp["gdn_a_log"][0][h], f32),
                 dtb=np.full((128, 1), inp["gdn_dt_bias"][0][h], f32),
                 normw=np.ascontiguousarray(np.broadcast_to(inp["gdn_norm"][0][None, :], (64, 128))))
        m.update(gc_)
        maps.append(m)
    resD = run(ncD, maps)
    yfull = np.concatenate([r["y"] for r in resD], axis=2)
    if dbg is not None:
        dbg["y"] = yfull
    ncE = cached(("E", T), lambda: build_E(T))
    x2s = shard_T(x2, S)
    ys = shard_T(yfull, S)
    maps = []
    for c in range(NCORE):
        m = {"xT": x2s[c], "yT": ys[c], "wo": inp["gdn_w_o"][0], "g3": vec128(inp["mix_norm"][3]),
             "w3": inp["moba_w_qkv"][1]}
        m.update(fm("f2", 2))
        maps.append(m)
    resE = run(ncE, maps)
    x3 = unshard_T([r["x3T"] for r in resE], S)
    qkv = unshard_T([r["qkvT"] for r in resE], S)
    if dbg is not None:
        dbg["ffn2"] = x3
    os_ = shard_T(moba_attn(qkv, S), S)
    ncG = cached(("G", T), lambda: build_G(T, True))
    x3s = shard_T(x3, S)
    maps = []
    for c in range(NCORE):
        m = {"xT": x3s[c], "oT": os_[c], "wo": inp["moba_w_o"][1], "gf": vec128(inp["final_norm"])}
        m.update(fm("f", 3))
        maps.append(m)
    resG = run(ncG, maps)
    return unshard_T([r["xoT"] for r in resG], S)


def kernel(**inputs):
    S = inputs["x"].shape[1]
    return forward(inputs, S).astype(np.float32)
```

```python
import math
from contextlib import ExitStack, contextmanager

import numpy as np
import ml_dtypes
import concourse.bass as bass
import concourse.mybir as mybir
from concourse.bass_utils import run_bass_kernel_spmd

F32 = mybir.dt.float32
BF16 = mybir.dt.bfloat16
AF = mybir.ActivationFunctionType
ALU = mybir.AluOpType
AX = mybir.AxisListType
NPBF = ml_dtypes.bfloat16

D = 1024
NH = 8
HD = 128
FFN = 2816
NCORE = 8
EPS = 1e-6
HALO = 128
BIG = 30000.0

SAME_ENGINE_SYNC = True
SELF_ORDERED = ("pe",)


class Eng:
    def __init__(self, name, e, sem):
        self.name, self.e, self.sem = name, e, sem
        self.cnt = 0
        self.seen = {}


class Buf:
    __slots__ = ("name", "t", "lw", "rds", "sem", "dcnt")

    def __init__(self, name, t=None):
        self.name, self.t = name, t
        self.lw = None
        self.rds = {}
        self.sem = None
        self.dcnt = 0

    def __getitem__(self, idx):
        return self.t[idx]


class Prog:
    def __init__(self, nc, es):
        self.nc, self.es, self.tes = nc, es, es
        self.eng = {}
        for name, e in (("pe", nc.tensor), ("dve", nc.vector), ("act", nc.scalar),
                        ("pool", nc.gpsimd), ("sp", nc.sync)):
            sem = es.enter_context(nc.semaphore("s_" + name))
            self.eng[name] = Eng(name, e, sem)
        self.bufs = []
        self.uid = 0
        self.free_sems = []

    def _name(self, name):
        self.uid += 1
        return "%s_%d" % (name, self.uid)

    def sbuf(self, name, shape, dt):
        name = self._name(name)
        t = self.tes.enter_context(self.nc.sbuf_tensor(name, list(shape), dt))
        b = Buf(name, t)
        self.bufs.append(b)
        return b

    def psum(self, name, shape, dt=F32):
        name = self._name(name)
        t = self.tes.enter_context(self.nc.psum_tensor(name, list(shape), dt))
        b = Buf(name, t)
        self.bufs.append(b)
        return b

    @contextmanager
    def scope(self):
        outer = self.tes
        nb = len(self.bufs)
        with ExitStack() as tes:
            self.tes = tes
            yield
            self.barrier()
        self.tes = outer

    def _wait(self, E, deps):
        best = {}
        for d in deps:
            if d is None:
                continue
            k, sem, v = d
            if k == E.name and (E.name in SELF_ORDERED or not SAME_ENGINE_SYNC):
                continue
            if E.seen.get(k, 0) >= v:
                continue
            if k not in best or best[k][2] < v:
                best[k] = d
        for k, (kk, sem, v) in best.items():
            E.e.wait_ge(sem, v)
            E.seen[k] = v

    @staticmethod
    def _deps(rd, wr):
        deps = []
        for b in rd:
            deps.append(b.lw)
        for b in wr:
            deps.append(b.lw)
            deps.extend(b.rds.values())
        return deps

    def op(self, en, fn, rd=(), wr=()):
        E = self.eng[en]
        self._wait(E, self._deps(rd, wr))
        ins = fn(E.e)
        E.cnt += 1
        ins.then_inc(E.sem, 1)
        ev = (E.name, E.sem, E.cnt)
        for b in rd:
            b.rds[E.name] = ev
        for b in wr:
            b.lw = ev
            b.rds = {}
        return ins

    def dma(self, qn, out, in_, rd=(), wr=(), **kw):
        Q = self.eng[qn]
        self._wait(Q, self._deps(rd, wr))
        b = (list(wr) + list(rd))[0]
        if b.sem is None:
            b.sem = self.es.enter_context(self.nc.semaphore("d_" + b.name))
        b.dcnt += 1
        Q.e.dma_start(out=out, in_=in_, **kw).then_inc(b.sem, 16)
        ev = ("d_" + b.name, b.sem, 16 * b.dcnt)
        for x in rd:
            x.rds[ev[0]] = ev
        for x in wr:
            x.lw = ev
            x.rds = {}

    def barrier(self, engines=("pe", "dve", "act", "pool", "sp")):
        evs = []
        for E in self.eng.values():
            if E.cnt:
                evs.append((E.name, E.sem, E.cnt))
        for b in self.bufs:
            if b.sem is not None and b.dcnt:
                evs.append(("d_" + b.name, b.sem, 16 * b.dcnt))
        for en in engines:
            E = self.eng[en]
            for (k, sem, v) in evs:
                if k == E.name or E.seen.get(k, 0) >= v:
                    continue
                E.e.wait_ge(sem, v)
                E.seen[k] = v

    def finish(self):
        self.barrier()


class Rot:
    def __init__(self, bufs):
        self.bufs, self.i = bufs, 0

    def get(self):
        b = self.bufs[self.i % len(self.bufs)]
        self.i += 1
        return b


def rot_sbuf(p, name, n, shape, dt):
    return Rot([p.sbuf(name, shape, dt) for _ in range(n)])


def rot_psum(p, name, n, shape, dt=F32):
    return Rot([p.psum(name, shape, dt) for _ in range(n)])


def load_small(p, name, ap, shape, dt=F32, q="sp"):
    b = p.sbuf(name, shape, dt)
    p.dma(q, b[:], ap, wr=[b])
    return b


def load_w(p, wb, w_ap, kch, n, stg):
    wv = w_ap.rearrange("(c p) n -> p c n", p=128)
    step = 2048
    for c in range(kch):
        for n0 in range(0, n, step):
            n1 = min(n, n0 + step)
            s = stg.get()
            p.dma("sp", s[:, 0:n1 - n0], wv[:, c, n0:n1], wr=[s])
            p.op("pool", lambda e: e.tensor_copy(out=wb[:, c, n0:n1], in_=s[:, 0:n1 - n0]), rd=[s], wr=[wb])
    return wb


def rmsnorm_fm(p, xs, N, gs, ones, sq, hs, rstd, ps):
    p.op("act", lambda e: e.activation(out=sq[:, :, 0:N], in_=xs[:, :, 0:N], func=AF.Square), rd=[xs], wr=[sq])
    for c in range(8):
        p.op("pe", lambda e: e.matmul(ps[:, 0:N], lhsT=ones[:], rhs=sq[:, c, 0:N], start=(c == 0), stop=(c == 7)),
             rd=[ones, sq], wr=[ps])
    p.op("act", lambda e: e.activation(out=rstd[:, 0:N], in_=ps[:, 0:N], func=AF.Sqrt, scale=1.0 / D, bias=EPS),
         rd=[ps], wr=[rstd])
    p.op("dve", lambda e: e.reciprocal(out=rstd[:, 0:N], in_=rstd[:, 0:N]), rd=[rstd], wr=[rstd])
    for c in range(8):
        p.op("dve", lambda e: e.scalar_tensor_tensor(out=hs[:, c, 0:N], in0=xs[:, c, 0:N], scalar=gs[:, c:c + 1],
                                                     in1=rstd[:, 0:N], op0=ALU.mult, op1=ALU.mult),
             rd=[xs, gs, rstd], wr=[hs])


def conv_fm(p, ub, N, K, cwb, j, carry, acc, first):
    wk = cwb[:, j, :]
    if first:
        p.op("pool", lambda e: e.memset(ub[:, 0:K - 1], 0.0), wr=[ub])
    else:
        p.op("pool", lambda e: e.tensor_copy(out=ub[:, 0:K - 1], in_=carry[:]), rd=[carry], wr=[ub])
    p.op("pool", lambda e: e.tensor_copy(out=carry[:], in_=ub[:, N:N + K - 1]), rd=[ub], wr=[carry])
    p.op("pool", lambda e: e.tensor_scalar(out=acc[:, 0:N], in0=ub[:, K - 1:K - 1 + N], scalar1=wk[:, K - 1:K],
                                           scalar2=None, op0=ALU.mult), rd=[ub, cwb], wr=[acc])
    for k in range(K - 1):
        p.op("dve", lambda e: e.scalar_tensor_tensor(out=acc[:, 0:N], in0=ub[:, k:k + N], scalar=wk[:, k:k + 1],
                                                     in1=acc[:, 0:N], op0=ALU.mult, op1=ALU.add),
             rd=[ub, acc, cwb], wr=[acc])


def tok_tiles(T, nt):
    tiles = []
    t = 0
    while t < T:
        n = min(nt, T - t)
        tiles.append((t, n))
        t += n
    return tiles


def pass_proj(p, T, xT, g_ap, w_ap, FO, outT, out_dt, nconv=0, K=0, cw_ap=None, scale_chunks=(), scale=1.0, NT=512):
    with p.scope():
        W = p.sbuf("W", [128, 8, FO], BF16)
        with p.scope():
            load_w(p, W, w_ap, 8, FO, rot_sbuf(p, "stg", 2, [128, 2048], F32))
        gs = load_small(p, "gs", g_ap, [128, 8])
        nch = (FO + 127) // 128
        if nconv:
            cw = load_small(p, "cw", cw_ap, [128, nconv, K])
            carry = [p.sbuf("carry", [128, K - 1], F32) for _ in range(nconv)]
            ubs = rot_sbuf(p, "ub", 2, [128, K - 1 + NT], F32)
            accs = rot_sbuf(p, "acc", 2, [128, NT], F32)
        ones = p.sbuf("ones", [128, 128], BF16)
        p.op("dve", lambda e: e.memset(ones[:], 1.0), wr=[ones])
        xsr = rot_sbuf(p, "xs", 2, [128, 8, NT], F32)
        sq = p.sbuf("sq", [128, 8, NT], BF16)
        hsr = rot_sbuf(p, "hs", 2, [128, 8, NT], BF16)
        rstd = p.sbuf("rstd", [128, NT], F32)
        obr = rot_sbuf(p, "ob", 3, [128, NT], out_dt)
        ps_ss = p.psum("ps_ss", [128, 512])
        psr = rot_psum(p, "ps", 3, [128, 512])
        xv = xT.rearrange("(c p) t -> p c t", p=128)
        for ti, (t0, N) in enumerate(tok_tiles(T, NT)):
            xs = xsr.get()
            p.dma("sp", xs[:, :, 0:N], xv[:, :, t0:t0 + N], wr=[xs])
            hs = hsr.get()
            rmsnorm_fm(p, xs, N, gs, ones, sq, hs, rstd, ps_ss)
            for j in range(nch):
                M = min(128, FO - j * 128)
                ps = psr.get()
                for c in range(8):
                    p.op("pe", lambda e: e.matmul(ps[0:M, 0:N], lhsT=W[:, c, j * 128:j * 128 + M], rhs=hs[:, c, 0:N],
                                                  start=(c == 0), stop=(c == 7)), rd=[W, hs], wr=[ps])
                ob = obr.get()
                if j < nconv:
                    ub = ubs.get()
                    acc = accs.get()
                    p.op("act", lambda e: e.activation(out=ub[:, K - 1:K - 1 + N], in_=ps[:, 0:N], func=AF.Copy),
                         rd=[ps], wr=[ub])
                    conv_fm(p, ub, N, K, cw, j, carry[j], acc, ti == 0)
                    p.op("act", lambda e: e.activation(out=ob[:, 0:N], in_=acc[:, 0:N], func=AF.Silu),
                         rd=[acc], wr=[ob])
                else:
                    sc = scale if j in scale_chunks else 1.0
                    p.op("act", lambda e: e.activation(out=ob[0:M, 0:N], in_=ps[0:M, 0:N], func=AF.Copy, scale=sc),
                         rd=[ps], wr=[ob])
                p.dma("pool", outT[j * 128:j * 128 + M, t0:t0 + N], ob[0:M, 0:N], rd=[ob])


def pass_oproj(p, T, xT, oT, o_dt, w_ap, xoutT, NT=512):
    with p.scope():
        W = p.sbuf("Wo", [128, 8, D], BF16)
        with p.scope():
            load_w(p, W, w_ap, 8, D, rot_sbuf(p, "stg", 2, [128, 2048], F32))
        xsr = rot_sbuf(p, "xs", 2, [128, 8, NT], F32)
        osr = rot_sbuf(p, "os", 2, [128, 8, NT], o_dt)
        if o_dt != BF16:
            obr = rot_sbuf(p, "obf", 2, [128, 8, NT], BF16)
        psr = rot_psum(p, "ps", 3, [128, 512])
        xv = xT.rearrange("(c p) t -> p c t", p=128)
        ov = oT.rearrange("(c p) t -> p c t", p=128)
        xov = xoutT.rearrange("(c p) t -> p c t", p=128)
        for ti, (t0, N) in enumerate(tok_tiles(T, NT)):
            xs = xsr.get()
            p.dma("sp", xs[:, :, 0:N], xv[:, :, t0:t0 + N], wr=[xs])
            os_ = osr.get()
            p.dma("sp", os_[:, :, 0:N], ov[:, :, t0:t0 + N], wr=[os_])
            if o_dt != BF16:
                ob = obr.get()
                p.op("pool", lambda e: e.tensor_copy(out=ob[:, :, 0:N], in_=os_[:, :, 0:N]), rd=[os_], wr=[ob])
                os_ = ob
            for fo in range(8):
                ps = psr.get()
                for c in range(8):
                    p.op("pe", lambda e: e.matmul(ps[:, 0:N], lhsT=W[:, c, fo * 128:(fo + 1) * 128], rhs=os_[:, c, 0:N],
                                                  start=(c == 0), stop=(c == 7)), rd=[W, os_], wr=[ps])
                p.op("dve", lambda e: e.tensor_tensor(out=xs[:, fo, 0:N], in0=xs[:, fo, 0:N], in1=ps[:, 0:N], op=ALU.add),
                     rd=[xs, ps], wr=[xs])
            p.dma("pool", xov[:, :, t0:t0 + N], xs[:, :, 0:N], rd=[xs])


def pass_ffn(p, T, xT, g_ap, wup_ap, cw_ap, wdn_ap, xoutT, final_g_ap=None, NT=256):
    NJ = FFN // 128
    with p.scope():
        Wu = p.sbuf("Wu", [128, 8, 2 * FFN], BF16)
        Wd = p.sbuf("Wd", [128, NJ, D], BF16)
        with p.scope():
            stg = rot_sbuf(p, "stg", 2, [128, 2048], F32)
            load_w(p, Wu, wup_ap, 8, 2 * FFN, stg)
            load_w(p, Wd, wdn_ap, NJ, D, stg)
        gs = load_small(p, "gs", g_ap, [128, 8])
        cw = load_small(p, "cw", cw_ap, [128, 2 * NJ, 3])
        if final_g_ap is not None:
            gf = load_small(p, "gf", final_g_ap, [128, 8])
            onesf = p.sbuf("onesf", [128, 128], F32)
            p.op("dve", lambda e: e.memset(onesf[:], 1.0), wr=[onesf])
            sqf = p.sbuf("sqf", [128, 8, NT], F32)
        carry = [p.sbuf("carry", [128, 2], F32) for _ in range(2 * NJ)]
        ones = p.sbuf("ones", [128, 128], BF16)
        p.op("dve", lambda e: e.memset(ones[:], 1.0), wr=[ones])
        xsr = rot_sbuf(p, "xs", 2, [128, 8, NT], F32)
        sq = p.sbuf("sq", [128, 8, NT], BF16)
        hs = p.sbuf("hs", [128, 8, NT], BF16)
        rstd = p.sbuf("rstd", [128, NT], F32)
        act = p.sbuf("actT", [128, NJ, NT], BF16)
        ubr = rot_sbuf(p, "ub", 4, [128, 2 + NT], F32)
        acr = rot_sbuf(p, "acc", 4, [128, NT], F32)
        sgr = rot_sbuf(p, "sg", 2, [128, NT], F32)
        ps_ss = p.psum("ps_ss", [128, 512])
        psr = rot_psum(p, "ps", 4, [128, 512])
        pso = rot_psum(p, "pso", 2, [128, 512])
        xv = xT.rearrange("(c p) t -> p c t", p=128)
        xov = xoutT.rearrange("(c p) t -> p c t", p=128)
        for ti, (t0, N) in enumerate(tok_tiles(T, NT)):
            xs = xsr.get()
            p.dma("sp", xs[:, :, 0:N], xv[:, :, t0:t0 + N], wr=[xs])
            rmsnorm_fm(p, xs, N, gs, ones, sq, hs, rstd, ps_ss)
            for j in range(NJ):
                accs = []
                for half in range(2):
                    col = half * FFN + j * 128
                    ps = psr.get()
                    for c in range(8):
                        p.op("pe", lambda e: e.matmul(ps[:, 0:N], lhsT=Wu[:, c, col:col + 128], rhs=hs[:, c, 0:N],
                                                      start=(c == 0), stop=(c == 7)), rd=[Wu, hs], wr=[ps])
                    ub = ubr.get()
                    acc = acr.get()
                    p.op("act", lambda e: e.activation(out=ub[:, 2:2 + N], in_=ps[:, 0:N], func=AF.Copy),
                         rd=[ps], wr=[ub])
                    conv_fm(p, ub, N, 3, cw, half * NJ + j, carry[half * NJ + j], acc, ti == 0)
                    accs.append(acc)
                sg = sgr.get()
                p.op("act", lambda e: e.activation(out=sg[:, 0:N], in_=accs[0][:, 0:N], func=AF.Silu),
                     rd=[accs[0]], wr=[sg])
                p.op("dve", lambda e: e.tensor_tensor(out=act[:, j, 0:N], in0=sg[:, 0:N], in1=accs[1][:, 0:N],
                                                      op=ALU.mult), rd=[sg, accs[1]], wr=[act])
            for fo in range(8):
                ps = pso.get()
                for j in range(NJ):
                    p.op("pe", lambda e: e.matmul(ps[:, 0:N], lhsT=Wd[:, j, fo * 128:(fo + 1) * 128], rhs=act[:, j, 0:N],
                                                  start=(j == 0), stop=(j == NJ - 1)), rd=[Wd, act], wr=[ps])
                p.op("dve", lambda e: e.tensor_tensor(out=xs[:, fo, 0:N], in0=xs[:, fo, 0:N], in1=ps[:, 0:N], op=ALU.add),
                     rd=[xs, ps], wr=[xs])
            if final_g_ap is not None:
                p.op("act", lambda e: e.activation(out=sqf[:, :, 0:N], in_=xs[:, :, 0:N], func=AF.Square),
                     rd=[xs], wr=[sqf])
                for c in range(8):
                    p.op("pe", lambda e: e.matmul(ps_ss[:, 0:N], lhsT=onesf[:], rhs=sqf[:, c, 0:N], start=(c == 0),
                                                  stop=(c == 7)), rd=[onesf, sqf], wr=[ps_ss])
                p.op("act", lambda e: e.activation(out=rstd[:, 0:N], in_=ps_ss[:, 0:N], func=AF.Sqrt, scale=1.0 / D,
                                                   bias=EPS), rd=[ps_ss], wr=[rstd])
                p.op("dve", lambda e: e.reciprocal(out=rstd[:, 0:N], in_=rstd[:, 0:N]), rd=[rstd], wr=[rstd])
                for c in range(8):
                    p.op("dve", lambda e: e.scalar_tensor_tensor(out=xs[:, c, 0:N], in0=xs[:, c, 0:N],
                                                                 scalar=gf[:, c:c + 1], in1=rstd[:, 0:N],
                                                                 op0=ALU.mult, op1=ALU.mult),
                         rd=[xs, gf, rstd], wr=[xs])
            p.dma("pool", xov[:, :, t0:t0 + N], xs[:, :, 0:N], rd=[xs])


def pass_sconv(p, T, xT, g_ap, win_ap, cw_ap, wout_ap, xoutT, NT=512):
    with p.scope():
        Wi = p.sbuf("Wi", [128, 8, 3 * D], BF16)
        Wo = p.sbuf("Wo", [128, 8, D], BF16)
        with p.scope():
            stg = rot_sbuf(p, "stg", 2, [128, 2048], F32)
            load_w(p, Wi, win_ap, 8, 3 * D, stg)
            load_w(p, Wo, wout_ap, 8, D, stg)
        gs = load_small(p, "gs", g_ap, [128, 8])
        cw = load_small(p, "cw", cw_ap, [128, 8, 3])
        carry = [p.sbuf("carry", [128, 2], F32) for _ in range(8)]
        ones = p.sbuf("ones", [128, 128], BF16)
        p.op("dve", lambda e: e.memset(ones[:], 1.0), wr=[ones])
        xsr = rot_sbuf(p, "xs", 2, [128, 8, NT], F32)
        sq = p.sbuf("sq", [128, 8, NT], BF16)
        hs = p.sbuf("hs", [128, 8, NT], BF16)
        rstd = p.sbuf("rstd", [128, NT], F32)
        ys = p.sbuf("ys", [128, 8, NT], BF16)
        ubr = rot_sbuf(p, "ub", 2, [128, 2 + NT], F32)
        acr = rot_sbuf(p, "acc", 2, [128, NT], F32)
        tmr = rot_sbuf(p, "tm", 2, [128, NT], F32)
        ps_ss = p.psum("ps_ss", [128, 512])
        psr = rot_psum(p, "ps", 6, [128, 512])
        xv = xT.rearrange("(c p) t -> p c t", p=128)
        xov = xoutT.rearrange("(c p) t -> p c t", p=128)
        for ti, (t0, N) in enumerate(tok_tiles(T, NT)):
            xs = xsr.get()
            p.dma("sp", xs[:, :, 0:N], xv[:, :, t0:t0 + N], wr=[xs])
            rmsnorm_fm(p, xs, N, gs, ones, sq, hs, rstd, ps_ss)
            for j in range(8):
                pss = []
                for part in range(3):
                    col = part * D + j * 128
                    ps = psr.get()
                    for c in range(8):
                        p.op("pe", lambda e: e.matmul(ps[:, 0:N], lhsT=Wi[:, c, col:col + 128], rhs=hs[:, c, 0:N],
                                                      start=(c == 0), stop=(c == 7)), rd=[Wi, hs], wr=[ps])
                    pss.append(ps)
                ub = ubr.get()
                acc = acr.get()
                tm = tmr.get()
                p.op("act", lambda e: e.activation(out=tm[:, 0:N], in_=pss[1][:, 0:N], func=AF.Copy),
                     rd=[pss[1]], wr=[tm])
                p.op("dve", lambda e: e.tensor_tensor(out=ub[:, 2:2 + N], in0=tm[:, 0:N], in1=pss[2][:, 0:N],
                                                      op=ALU.mult), rd=[tm, pss[2]], wr=[ub])
                conv_fm(p, ub, N, 3, cw, j, carry[j], acc, ti == 0)
                p.op("dve", lambda e: e.tensor_tensor(out=ys[:, j, 0:N], in0=acc[:, 0:N], in1=pss[0][:, 0:N],
                                                      op=ALU.mult), rd=[acc, pss[0]], wr=[ys])
            for fo in range(8):
                ps = psr.get()
                for c in range(8):
                    p.op("pe", lambda e: e.matmul(ps[:, 0:N], lhsT=Wo[:, c, fo * 128:(fo + 1) * 128], rhs=ys[:, c, 0:N],
                                                  start=(c == 0), stop=(c == 7)), rd=[Wo, ys], wr=[ps])
                p.op("dve", lambda e: e.tensor_tensor(out=xs[:, fo, 0:N], in0=xs[:, fo, 0:N], in1=ps[:, 0:N], op=ALU.add),
                     rd=[xs, ps], wr=[xs])
            p.dma("pool", xov[:, :, t0:t0 + N], xs[:, :, 0:N], rd=[xs])


def moba_attention(p, S, nb, qT, kT, v, oT, ident_ap, esel_ap, cmask_ap, ktab_ap, qrows_ap):
    NB = S // 256
    NQT = S // 512
    NKC = S // 128
    with p.scope():
        ident = load_small(p, "ident", ident_ap, [128, 128], BF16)
        esel = load_small(p, "esel", esel_ap, [128, 64, 128], BF16)
        cmask = load_small(p, "cmask", cmask_ap, [128, 4, 512], BF16)
        ktab = load_small(p, "ktab", ktab_ap, [128, 128], F32)
        onesf = p.sbuf("onesf", [128, 128], F32)
        p.op("dve", lambda e: e.memset(onesf[:], 1.0), wr=[onesf])
        kTs = p.sbuf("kTs", [128, S], BF16)
        qTs = p.sbuf("qTs", [128, S], BF16)
        vs = p.sbuf("vs", [128, NKC, 128], BF16)
        kmf = p.sbuf("kmf", [128, 64], F32)
        kmb = p.sbuf("kmb", [128, 64], BF16)
        bTr = rot_sbuf(p, "biasT", 2, [128, 512], BF16)
        for b_ in bTr.bufs:
            p.op("pool", lambda e: e.memset(b_[:], 0.0), wr=[b_])
            p.dma("sp", b_[64:66, :], qrows_ap, wr=[b_])
        gsbr = rot_sbuf(p, "gsb", 2, [128, 64], F32)
        t8r = rot_sbuf(p, "top8", 2, [128, 8], F32)
        mskr = rot_sbuf(p, "msk", 2, [128, 64], BF16)
        PTr = rot_sbuf(p, "PT", 3, [128, 512], BF16)
        raccr = rot_sbuf(p, "racc", 2, [128, 512], F32)
        rinv = p.sbuf("rinv", [128, 512], F32)
        otr = rot_sbuf(p, "ot", 2, [128, 512], BF16)
        psSr = rot_psum(p, "psS", 3, [128, 512])
        psOr = rot_psum(p, "psO", 2, [128, 512])
        psg = p.psum("psg", [128, 64])
        pst = p.psum("pst", [64, 128], BF16)
        psr_ = p.psum("psr", [128, 512])
        for b in range(nb):
            p.dma("sp", kTs[:], kT[b], wr=[kTs])
            p.dma("sp", qTs[:], qT[b], wr=[qTs])
            p.dma("sp", vs[:], v[b].rearrange("(c p) d -> p c d", p=128), wr=[vs])
            p.op("pool", lambda e: e.memset(kmf[:], 0.0), wr=[kmf])
            p.op("dve", lambda e: e.tensor_reduce(out=kmf[:, 0:NB], in_=kTs[:].rearrange("p (n k) -> p n k", k=256),
                                                  axis=AX.X, op=ALU.add), rd=[kTs], wr=[kmf])
            p.op("dve", lambda e: e.tensor_copy(out=kmb[:], in_=kmf[:]), rd=[kmf], wr=[kmb])
            for qt in range(NQT):
                t0 = qt * 512
                bT = bTr.get()
                for s in range(4):
                    own = 2 * qt + s // 2
                    msk = mskr.get()
                    if own == 0:
                        p.op("pool", lambda e: e.memset(msk[:], 0.0), wr=[msk])
                    else:
                        p.op("pe", lambda e: e.matmul(psg[:], lhsT=qTs[:, t0 + 128 * s:t0 + 128 * s + 128], rhs=kmb[:],
                                                      start=True, stop=True), rd=[qTs, kmb], wr=[psg])
                        gsb = gsbr.get()
                        p.op("pool", lambda e: e.memset(gsb[:], -1e30), wr=[gsb])
                        p.op("act", lambda e: e.activation(out=gsb[:, 0:own], in_=psg[:, 0:own], func=AF.Copy),
                             rd=[psg], wr=[gsb])
                        t8 = t8r.get()
                        p.op("dve", lambda e: e.max(out=t8[:], in_=gsb[:]), rd=[gsb], wr=[t8])
                        p.op("dve", lambda e: e.tensor_scalar(out=msk[:], in0=gsb[:], scalar1=t8[:, 2:3], scalar2=-1.0,
                                                              op0=ALU.is_ge, op1=ALU.add), rd=[gsb, t8], wr=[msk])
                        hi = min(64, own + 2)
                        p.op("pool", lambda e: e.memset(msk[:, own:hi], 0.0), wr=[msk])
                    p.op("pe", lambda e: e.transpose(out=pst[:], in_=msk[:], identity=ident[:]), rd=[msk, ident], wr=[pst])
                    p.op("act", lambda e: e.activation(out=bT[0:64, 128 * s:128 * s + 128], in_=pst[:], func=AF.Copy),
                         rd=[pst], wr=[bT])
                psO = psOr.get()
                racc = raccr.get()
                nkc = 4 * qt + 4

                def emit_S(kc):
                    n = kc // 2
                    d = kc - 4 * qt
                    psS = psSr.get()
                    p.op("pe", lambda e: e.matmul(psS[:], lhsT=kTs[:, kc * 128:kc * 128 + 128], rhs=qTs[:, t0:t0 + 512],
                                                  start=True, stop=False), rd=[kTs, qTs], wr=[psS])
                    p.op("pe", lambda e: e.matmul(psS[:], lhsT=esel[:, n, :], rhs=bT[:], start=False, stop=(d < 0)),
                         rd=[esel, bT], wr=[psS])
                    if d >= 0:
                        p.op("pe", lambda e: e.matmul(psS[:], lhsT=ident[:], rhs=cmask[:, d, :], start=False, stop=True),
                             rd=[ident, cmask], wr=[psS])
                    return psS

                psS_next = emit_S(0)
                for kc in range(nkc):
                    psS = psS_next
                    if kc + 1 < nkc:
                        psS_next = emit_S(kc + 1)
                    PT = PTr.get()
                    m = 4 * qt + 3 - kc
                    p.op("act", lambda e: e.activation(out=PT[:], in_=psS[:], func=AF.Exp, bias=ktab[:, m:m + 1], scale=1.0),
                         rd=[psS, ktab], wr=[PT])
                    p.op("pe", lambda e: e.matmul(psO[:], lhsT=vs[:, kc, :], rhs=PT[:], start=(kc == 0),
                                                  stop=(kc == nkc - 1)), rd=[vs, PT], wr=[psO])
                    if kc == 0:
                        p.op("dve", lambda e: e.tensor_copy(out=racc[:], in_=PT[:]), rd=[PT], wr=[racc])
                    else:
                        p.op("dve", lambda e: e.tensor_tensor(out=racc[:], in0=racc[:], in1=PT[:], op=ALU.add),
                             rd=[racc, PT], wr=[racc])
                p.op("pe", lambda e: e.matmul(psr_[:], lhsT=onesf[:], rhs=racc[:], start=True, stop=True),
                     rd=[onesf, racc], wr=[psr_])
                p.op("dve", lambda e: e.reciprocal(out=rinv[:], in_=psr_[:]), rd=[psr_], wr=[rinv])
                ot = otr.get()
                p.op("dve", lambda e: e.tensor_tensor(out=ot[:], in0=psO[:], in1=rinv[:], op=ALU.mult),
                     rd=[psO, rinv], wr=[ot])
                p.dma("pool", oT[b][:, t0:t0 + 512], ot[:], rd=[ot])


def new_nc():
    return bass.Bass("TRN2", target_bir_lowering=False)


def din(nc, name, shape, dt=F32):
    return nc.dram_tensor(name, list(shape), dt, kind="ExternalInput").ap()


def dout(nc, name, shape, dt=F32):
    return nc.dram_tensor(name, list(shape), dt, kind="ExternalOutput").ap()


def dtmp(nc, name, shape, dt=F32):
    return nc.dram_tensor(name, list(shape), dt, kind="Internal").ap()


def ffn_inputs(nc, tag):
    return dict(g=din(nc, tag + "_g", [128, 8]), wup=din(nc, tag + "_wup", [D, 2 * FFN]),
                cw=din(nc, tag + "_cw", [128, 2 * (FFN // 128), 3]), wdn=din(nc, tag + "_wdn", [FFN, D]))


def build_A(T):
    nc = new_nc()
    xT = din(nc, "xT", [D, T])
    g = din(nc, "g", [128, 8])
    w = din(nc, "w", [D, 3 * D])
    o = dout(nc, "qkvT", [3 * D, T], BF16)
    with ExitStack() as es:
        p = Prog(nc, es)
        pass_proj(p, T, xT, g, w, 3 * D, o, BF16, scale_chunks=range(8), scale=HD ** -0.5)
        p.finish()
    return nc


def build_B(S):
    nc = new_nc()
    qT = din(nc, "qT", [2, 128, S], BF16)
    kT = din(nc, "kT", [2, 128, S], BF16)
    v = din(nc, "v", [2, S, 128], BF16)
    ident = din(nc, "ident", [128, 128], BF16)
    esel = din(nc, "esel", [128, 64, 128], BF16)
    cmask = din(nc, "cmask", [128, 4, 512], BF16)
    ktab = din(nc, "ktab", [128, 128])
    qrows = din(nc, "qrows", [2, 512], BF16)
    oT = dout(nc, "oT", [2, 128, S], BF16)
    with ExitStack() as es:
        p = Prog(nc, es)
        moba_attention(p, S, 2, qT, kT, v, oT, ident, esel, cmask, ktab, qrows)
        p.finish()
    return nc


def build_G(T, final):
    nc = new_nc()
    xT = din(nc, "xT", [D, T])
    oT = din(nc, "oT", [D, T], BF16)
    wo = din(nc, "wo", [D, D])
    f = ffn_inputs(nc, "f")
    gf = din(nc, "gf", [128, 8]) if final else None
    xm = dtmp(nc, "xm", [D, T])
    xo = dout(nc, "xoT", [D, T])
    with ExitStack() as es:
        p = Prog(nc, es)
        pass_oproj(p, T, xT, oT, BF16, wo, xm)
        pass_ffn(p, T, xm, f["g"], f["wup"], f["cw"], f["wdn"], xo, final_g_ap=gf)
        p.finish()
    return nc


def run(nc, in_maps):
    res = run_bass_kernel_spmd(nc, in_maps, core_ids=list(range(NCORE)))
    return res.results


def shard_T(full, S):
    TQ = S // 4
    outs = []
    for c in range(NCORE):
        b, qd = divmod(c, 4)
        lo = qd * TQ - HALO
        if lo < 0:
            blk = np.concatenate([np.zeros((HALO, full.shape[2]), full.dtype), full[b, 0:TQ]], axis=0)
        else:
            blk = full[b, lo:lo + HALO + TQ]
        outs.append(np.ascontiguousarray(blk.T))
    return outs


def unshard_T(parts, S):
    TQ = S // 4
    F = parts[0].shape[0]
    full = np.empty((2, S, F), parts[0].dtype)
    for c in range(NCORE):
        b, qd = divmod(c, 4)
        full[b, qd * TQ:(qd + 1) * TQ] = parts[c][:, HALO:].T
    return full


def vec128(v):
    return np.ascontiguousarray(v.reshape(-1, 128).T)


def conv128(w):
    K, C = w.shape
    return np.ascontiguousarray(w.T.reshape(C // 128, 128, K).transpose(1, 0, 2))


def ffn_maps(tag, g, wup, cw, wdn):
    return {tag + "_g": vec128(g), tag + "_wup": wup, tag + "_cw": conv128(cw), tag + "_wdn": wdn}


def moba_consts(h):
    slope = 2.0 ** (-(h + 1))
    ident = np.eye(128, dtype=np.float32).astype(NPBF)
    esel = np.zeros((128, 64, 128), np.float32)
    for n in range(64):
        esel[n, n, :] = BIG
    esel[64:66, :, :] = 1.0
    j = np.arange(128)[:, None]
    i = np.arange(512)[None, :]
    cmask = np.zeros((128, 4, 512), np.float32)
    for d in range(4):
        cmask[:, d, :] = np.where(128 * d + j <= i, 0.0, -BIG)
    m = np.arange(128)[None, :]
    ktab = (slope * (j - 127 - 128 * m)).astype(np.float32)
    r = 511 - np.arange(512)
    qrows = np.stack([slope * (r // 4 * 4), slope * (r % 4)]).astype(np.float32)
    return dict(ident=ident, esel=esel.astype(NPBF), cmask=cmask.astype(NPBF), ktab=ktab, qrows=qrows.astype(NPBF))


_CACHE = {}


def cached(key, fn):
    if key not in _CACHE:
        _CACHE[key] = fn()
    return _CACHE[key]


def moba_layer_mix(x_full, S, g, wqkv):
    T = HALO + S // 4
    ncA = cached(("A", T), lambda: build_A(T))
    xs = shard_T(x_full, S)
    resA = run(ncA, [{"xT": xs[c], "g": vec128(g), "w": wqkv} for c in range(NCORE)])
    qkv = unshard_T([r["qkvT"] for r in resA], S)
    ncB = cached(("B", S), lambda: build_B(S))
    maps = []
    for h in range(NH):
        m = dict(qT=np.ascontiguousarray(qkv[:, :, h * 128:(h + 1) * 128].transpose(0, 2, 1)),
                 kT=np.ascontiguousarray(qkv[:, :, D + h * 128:D + (h + 1) * 128].transpose(0, 2, 1)),
                 v=np.ascontiguousarray(qkv[:, :, 2 * D + h * 128:2 * D + (h + 1) * 128]))
        m.update(moba_consts(h))
        maps.append(m)
    resB = run(ncB, maps)
    o_full = np.concatenate([r["oT"].transpose(0, 2, 1) for r in resB], axis=2)
    return xs, shard_T(o_full, S)


def gdn_scan(p, S, q, k, v, z, atab, btab, alog_ap, dtb_ap, normw_ap, U_ap, SL_ap, SU_ap, UI_ap, id64_ap, y):
    NCH = S // 64
    G = 8
    NG = NCH // G
    with p.scope():
        U = load_small(p, "U", U_ap, [64, 64])
        SL = load_small(p, "SL", SL_ap, [64, 64])
        SU = load_small(p, "SU", SU_ap, [64, 64])
        UI = load_small(p, "UI", UI_ap, [64, 64])
        id64 = load_small(p, "id64", id64_ap, [64, 64])
        normw = load_small(p, "normw", normw_ap, [64, 128])
        alog = load_small(p, "alog", alog_ap, [128, 1])
        dtb = load_small(p, "dtb", dtb_ap, [128, 1])
        ones64 = p.sbuf("ones64", [64, 128], F32)
        p.op("dve", lambda e: e.memset(ones64[:], 1.0), wr=[ones64])
        nega = p.sbuf("nega", [128, 1], F32)
        p.op("act", lambda e: e.activation(out=nega[:], in_=alog[:], func=AF.Exp), rd=[alog], wr=[nega])
        p.op("dve", lambda e: e.tensor_scalar(out=nega[:], in0=nega[:], scalar1=-1.0, scalar2=None, op0=ALU.mult),
             rd=[nega], wr=[nega])
        gtab, betab = [], []
        for b in range(2):
            at = load_small(p, "at", atab[b], [64, NCH])
            bt = load_small(p, "bt", btab[b], [64, NCH])
            xx = p.sbuf("xx", [64, NCH], F32)
            ax = p.sbuf("ax", [64, NCH], F32)
            gt = p.sbuf("gt", [64, NCH], F32)
            be = p.sbuf("be", [64, NCH], F32)
            p.op("dve", lambda e: e.tensor_scalar(out=xx[:], in0=at[:], scalar1=dtb[0:64, 0:1], scalar2=None, op0=ALU.add),
                 rd=[at, dtb], wr=[xx])
            p.op("dve", lambda e: e.tensor_scalar(out=ax[:], in0=xx[:], scalar1=-1.0, scalar2=None, op0=ALU.mult),
                 rd=[xx], wr=[ax])
            p.op("dve", lambda e: e.tensor_tensor(out=ax[:], in0=ax[:], in1=xx[:], op=ALU.min), rd=[ax, xx], wr=[ax])
            p.op("act", lambda e: e.activation(out=ax[:], in_=ax[:], func=AF.Exp), rd=[ax], wr=[ax])
            p.op("act", lambda e: e.activation(out=ax[:], in_=ax[:], func=AF.Ln, bias=1.0, scale=1.0), rd=[ax], wr=[ax])
            p.op("dve", lambda e: e.tensor_scalar(out=xx[:], in0=xx[:], scalar1=0.0, scalar2=None, op0=ALU.max),
                 rd=[xx], wr=[xx])
            p.op("dve", lambda e: e.tensor_tensor(out=xx[:], in0=xx[:], in1=ax[:], op=ALU.add), rd=[xx, ax], wr=[xx])
            p.op("dve", lambda e: e.tensor_scalar(out=gt[:], in0=xx[:], scalar1=nega[0:64, 0:1], scalar2=None, op0=ALU.mult),
                 rd=[xx, nega], wr=[gt])
            p.op("act", lambda e: e.activation(out=be[:], in_=bt[:], func=AF.Exp, scale=-1.0), rd=[bt], wr=[be])
            p.op("dve", lambda e: e.tensor_scalar(out=be[:], in0=be[:], scalar1=1.0, scalar2=None, op0=ALU.add),
                 rd=[be], wr=[be])
            p.op("dve", lambda e: e.reciprocal(out=be[:], in_=be[:]), rd=[be], wr=[be])
            gtab.append(gt)
            betab.append(be)

        rots = {}

        DEEP = {"u": 6, "wT": 6, "qdT": 6, "attnT": 6, "kd": 6, "sdbc": 8, "egc": 8, "kdsc": 8}

        def Tm(name, shape, n=2):
            n = max(n, DEEP.get(name, 0))
            if name not in rots:
                rots[name] = rot_sbuf(p, name, n, shape, F32)
            return rots[name].get()

        PS = rot_psum(p, "ps", 7, [128, 512])
        Sst = [[p.sbuf("S", [128, 128], F32) for _ in range(2)] for _ in range(2)]
        for b in range(2):
            p.op("pool", lambda e: e.memset(Sst[b][0][:], 0.0), wr=[Sst[b][0]])
        gq = [rot_sbuf(p, "gq", 2, [64, G, 128], F32) for _ in range(2)]
        gk = [rot_sbuf(p, "gk", 2, [64, G, 128], F32) for _ in range(2)]
        gv = [rot_sbuf(p, "gv", 2, [64, G, 128], F32) for _ in range(2)]
        gz = [rot_sbuf(p, "gz", 2, [64, G, 128], F32) for _ in range(2)]
        gy = [rot_sbuf(p, "gy", 2, [64, G, 128], F32) for _ in range(2)]

        def mm(out_ap, ps, lhsT, rhs, rd, start=True, stop=True):
            p.op("pe", lambda e: e.matmul(out_ap, lhsT=lhsT, rhs=rhs, start=start, stop=stop), rd=rd, wr=[ps])

        def evac(eng, name, shape, ps, ps_ap):
            t = Tm(name, shape)
            if eng == "act":
                p.op("act", lambda e: e.activation(out=t[:], in_=ps_ap, func=AF.Copy), rd=[ps], wr=[t])
            else:
                p.op("dve", lambda e: e.tensor_copy(out=t[:], in_=ps_ap), rd=[ps], wr=[t])
            return t

        def tscal(eng, name, shape, src, src_ap, sc, sc_ap, s2=None):
            t = Tm(name, shape)
            if s2 is None:
                p.op(eng, lambda e: e.tensor_scalar(out=t[:], in0=src_ap, scalar1=sc_ap, scalar2=None, op0=ALU.mult),
                     rd=[src, sc], wr=[t])
            else:
                p.op(eng, lambda e: e.tensor_scalar(out=t[:], in0=src_ap, scalar1=sc_ap, scalar2=s2, op0=ALU.mult,
                                                    op1=ALU.mult), rd=[src, sc], wr=[t])
            return t

        def rnorm(src, src_ap, scale_in):
            junk = Tm("junk", [64, 128])
            ss = Tm("ss", [64, 1], 4)
            p.op("dve", lambda e: e.scalar_tensor_tensor(out=junk[:], in0=src_ap, scalar=1.0, in1=src_ap, op0=ALU.mult,
                                                         op1=ALU.mult, accum_out=ss[:]), rd=[src], wr=[junk, ss])
            lg = Tm("lg", [64, 1], 4)
            p.op("act", lambda e: e.activation(out=lg[:], in_=ss[:], func=AF.Ln, bias=EPS, scale=scale_in), rd=[ss], wr=[lg])
            r = Tm("rr", [64, 1], 4)
            p.op("act", lambda e: e.activation(out=r[:], in_=lg[:], func=AF.Exp, scale=-0.5), rd=[lg], wr=[r])
            return r

        def pre(b, ci, qg, kg, vg, zg, yg, gi):
            gtb, beb = gtab[b], betab[b]
            gcol = gtb[:, ci:ci + 1]
            bcol = beb[:, ci:ci + 1]
            qa, ka, va, za = qg[:, gi, :], kg[:, gi, :], vg[:, gi, :], zg[:, gi, :]
            rq = rnorm(qg, qa, 1.0)
            qn = tscal("dve", "qn", [64, 128], qg, qa, rq, rq[:, 0:1], s2=HD ** -0.5)
            rk = rnorm(kg, ka, 1.0)
            kn = tscal("dve", "kn", [64, 128], kg, ka, rk, rk[:, 0:1])
            kb = tscal("pool", "kb", [64, 128], kn, kn[:], beb, bcol)
            vb = tscal("pool", "vb", [64, 128], vg, va, beb, bcol)
            ps = PS.get()
            mm(ps[0:64, 0:1], ps, U[:], gcol, [U, gtb])
            GC = evac("act", "GC", [64, 1], ps, ps[0:64, 0:1])
            gb = tscal("pool", "gb", [64, 128], ones64, ones64[:], gtb, gcol)
            ps = PS.get()
            mm(ps[:, 0:64], ps, gb[:], U[:], [gb, U])
            GR = evac("act", "GR", [128, 64], ps, ps[:, 0:64])
            egc = Tm("egc", [64, 1], 4)
            p.op("act", lambda e: e.activation(out=egc[:], in_=GC[:], func=AF.Exp), rd=[GC], wr=[egc])
            sdbc = Tm("sdbc", [128, 1], 4)
            p.op("act", lambda e: e.activation(out=sdbc[:], in_=GR[:, 63:64], func=AF.Exp), rd=[GR], wr=[sdbc])
            kdsc = Tm("kdsc", [64, 1], 4)
            p.op("act", lambda e: e.activation(out=kdsc[:], in_=GC[:], func=AF.Exp, scale=-1.0, bias=GR[0:64, 63:64]),
                 rd=[GC, GR], wr=[kdsc])
            dm = Tm("dm", [64, 64])
            p.op("dve", lambda e: e.tensor_scalar(out=dm[:], in0=GR[0:64, :], scalar1=GC[:, 0:1], scalar2=0.0,
                                                  op0=ALU.subtract, op1=ALU.min), rd=[GR, GC], wr=[dm])
            decT = Tm("decT", [64, 64])
            p.op("act", lambda e: e.activation(out=decT[:], in_=dm[:], func=AF.Exp), rd=[dm], wr=[decT])
            dx = Tm("dx", [64, 64])
            p.op("dve", lambda e: e.tensor_scalar(out=dx[:], in0=GR[0:64, :], scalar1=GC[:, 0:1], scalar2=0.0,
                                                  op0=ALU.subtract, op1=ALU.max), rd=[GR, GC], wr=[dx])
            dec = Tm("dec", [64, 64])
            p.op("act", lambda e: e.activation(out=dec[:], in_=dx[:], func=AF.Exp, scale=-1.0), rd=[dx], wr=[dec])
            dSU = Tm("dSU", [64, 64])
            p.op("pool", lambda e: e.tensor_tensor(out=dSU[:], in0=decT[:], in1=SU[:], op=ALU.mult), rd=[decT, SU], wr=[dSU])
            dUI = Tm("dUI", [64, 64])
            p.op("pool", lambda e: e.tensor_tensor(out=dUI[:], in0=decT[:], in1=UI[:], op=ALU.mult), rd=[decT, UI], wr=[dUI])
            dSL = Tm("dSL", [64, 64])
            p.op("pool", lambda e: e.tensor_tensor(out=dSL[:], in0=dec[:], in1=SL[:], op=ALU.mult), rd=[dec, SL], wr=[dSL])
            kbg = tscal("pool", "kbg", [64, 128], kb, kb[:], egc, egc[:, 0:1])
            kd = tscal("pool", "kd", [64, 128], kn, kn[:], kdsc, kdsc[:, 0:1])
            qd = tscal("pool", "qd", [64, 128], qn, qn[:], egc, egc[:, 0:1])
            tr = {}
            for nm, src in (("knT", kn), ("kbT", kb), ("qnT", qn), ("qdT", qd)):
                ps = PS.get()
                p.op("pe", lambda e: e.transpose(out=ps[:, 0:64], in_=src[:], identity=id64[:]), rd=[src, id64], wr=[ps])
                tr[nm] = evac("act" if nm in ("knT", "qnT") else "dve", nm, [128, 64], ps, ps[:, 0:64])
            knT, kbT, qnT, qdT = tr["knT"], tr["kbT"], tr["qnT"], tr["qdT"]

            def masked(nm, lhsT, rhs, msk):
                ps = PS.get()
                mm(ps[0:64, 0:64], ps, lhsT[:], rhs[:], [lhsT, rhs])
                t = Tm(nm, [64, 64])
                p.op("dve", lambda e: e.tensor_tensor(out=t[:], in0=ps[0:64, 0:64], in1=msk[:], op=ALU.mult),
                     rd=[ps, msk], wr=[t])
                return t

            A = masked("A", kbT, knT, dSL)
            B = masked("B", knT, kbT, dSU)
            attnT = masked("attnT", knT, qnT, dUI)
            R = Tm("R", [64, 64], 3)
            p.op("pool", lambda e: e.tensor_tensor(out=R[:], in0=id64[:], in1=B[:], op=ALU.subtract), rd=[id64, B], wr=[R])
            Pp, Qp = A, B
            for kk in range(1, 6):
                ps = PS.get()
                mm(ps[0:64, 0:64], ps, Qp[:], Pp[:], [Qp, Pp])
                Pk = evac("act", "Pk", [64, 64], ps, ps[0:64, 0:64])
                if kk < 5:
                    ps = PS.get()
                    mm(ps[0:64, 0:64], ps, Pp[:], Qp[:], [Pp, Qp])
                    Qk = evac("dve", "Qk", [64, 64], ps, ps[0:64, 0:64])
                ps = PS.get()
                mm(ps[0:64, 0:64], ps, Pk[:], R[:], [Pk, R])
                Rn = Tm("R", [64, 64], 3)
                p.op("dve", lambda e: e.tensor_tensor(out=Rn[:], in0=R[:], in1=ps[0:64, 0:64], op=ALU.add),
                     rd=[R, ps], wr=[Rn])
                R = Rn
                Pp = Pk
                if kk < 5:
                    Qp = Qk
            ps = PS.get()
            mm(ps[0:64, 0:128], ps, R[:], vb[:], [R, vb])
            u = evac("act", "u", [64, 128], ps, ps[0:64, 0:128])
            ps = PS.get()
            mm(ps[:, 0:64], ps, kbg[:], R[:], [kbg, R])
            wT = evac("act", "wT", [128, 64], ps, ps[:, 0:64])
            return dict(u=u, wT=wT, qdT=qdT, attnT=attnT, kd=kd, sdbc=sdbc, za=za, zg=zg, yg=yg, gi=gi)

        def scan(b, ci, d):
            u, wT, qdT, attnT, kd, sdbc = d['u'], d['wT'], d['qdT'], d['attnT'], d['kd'], d['sdbc']
            za, zg, yg, gi = d['za'], d['zg'], d['yg'], d['gi']
            Sc = Sst[b][ci % 2]
            Sn = Sst[b][(ci + 1) % 2]
            ps = PS.get()
            mm(ps[0:64, 0:128], ps, wT[:], Sc[:], [wT, Sc])
            vnew = Tm("vnew", [64, 128])
            p.op("dve", lambda e: e.tensor_tensor(out=vnew[:], in0=u[:], in1=ps[0:64, 0:128], op=ALU.subtract),
                 rd=[u, ps], wr=[vnew])
            ps3 = PS.get()
            mm(ps3[:, 0:128], ps3, kd[:], vnew[:], [kd, vnew])
            p.op("dve", lambda e: e.scalar_tensor_tensor(out=Sn[:], in0=Sc[:], scalar=sdbc[:, 0:1], in1=ps3[:, 0:128],
                                                         op0=ALU.mult, op1=ALU.add), rd=[Sc, sdbc, ps3], wr=[Sn])
            ps2 = PS.get()
            mm(ps2[0:64, 0:128], ps2, qdT[:], Sc[:], [qdT, Sc], start=True, stop=False)
            mm(ps2[0:64, 0:128], ps2, attnT[:], vnew[:], [attnT, vnew], start=False, stop=True)
            o = evac("act", "o", [64, 128], ps2, ps2[0:64, 0:128])
            ro = rnorm(o, o[:], 1.0 / HD)
            y1 = tscal("pool", "y1", [64, 128], o, o[:], ro, ro[:, 0:1])
            y2 = Tm("y2", [64, 128])
            p.op("pool", lambda e: e.tensor_tensor(out=y2[:], in0=y1[:], in1=normw[:], op=ALU.mult), rd=[y1, normw], wr=[y2])
            ez = Tm("ez", [64, 128])
            p.op("act", lambda e: e.activation(out=ez[:], in_=za, func=AF.Exp, scale=-1.0), rd=[zg], wr=[ez])
            p.op("pool", lambda e: e.tensor_scalar(out=ez[:], in0=ez[:], scalar1=1.0, scalar2=None, op0=ALU.add),
                 rd=[ez], wr=[ez])
            p.op("dve", lambda e: e.reciprocal(out=ez[:], in_=ez[:]), rd=[ez], wr=[ez])
            p.op("pool", lambda e: e.tensor_tensor(out=ez[:], in0=ez[:], in1=za, op=ALU.mult), rd=[ez, zg], wr=[ez])
            p.op("dve", lambda e: e.tensor_tensor(out=yg[:, gi, :], in0=y2[:], in1=ez[:], op=ALU.mult), rd=[y2, ez], wr=[yg])

        for g in range(NG):
            cur = []
            for b in range(2):
                sl = slice(g * G * 64, (g + 1) * G * 64)
                bufs = []
                for pool_, src in ((gq, q), (gk, k), (gv, v), (gz, z)):
                    t = pool_[b].get()
                    p.dma("sp", t[:], src[b][sl, :].rearrange("(g p) d -> p g d", p=64), wr=[t])
                    bufs.append(t)
                bufs.append(gy[b].get())
                cur.append(bufs)
            pend = [pre(b, g * G, *cur[b], 0) for b in range(2)]
            for gi in range(G):
                nxt = None
                if gi + 1 < G:
                    nxt = [pre(b, g * G + gi + 1, *cur[b], gi + 1) for b in range(2)]
                for b in range(2):
                    scan(b, g * G + gi, pend[b])
                pend = nxt
            for b in range(2):
                sl = slice(g * G * 64, (g + 1) * G * 64)
                yg = cur[b][4]
                p.dma("pool", y[b][sl, :].rearrange("(g p) d -> p g d", p=64), yg[:], rd=[yg])


def build_D(S):
    nc = new_nc()
    q = din(nc, "q", [2, S, 128])
    k = din(nc, "k", [2, S, 128])
    v = din(nc, "v", [2, S, 128])
    z = din(nc, "z", [2, S, 128])
    atab = din(nc, "atab", [2, 64, S // 64])
    btab = din(nc, "btab", [2, 64, S // 64])
    alog = din(nc, "alog", [128, 1])
    dtb = din(nc, "dtb", [128, 1])
    normw = din(nc, "normw", [64, 128])
    U = din(nc, "U", [64, 64])
    SL = din(nc, "SL", [64, 64])
    SU = din(nc, "SU", [64, 64])
    UI = din(nc, "UI", [64, 64])
    id64 = din(nc, "id64", [64, 64])
    y = dout(nc, "y", [2, S, 128])
    with ExitStack() as es:
        p = Prog(nc, es)
        gdn_scan(p, S, q, k, v, z, atab, btab, alog, dtb, normw, U, SL, SU, UI, id64, y)
        p.finish()
    return nc


def build_C(T):
    nc = new_nc()
    xT = din(nc, "xT", [D, T])
    oT = din(nc, "oT", [D, T], BF16)
    wo = din(nc, "wo", [D, D])
    f0 = ffn_inputs(nc, "f0")
    g1 = din(nc, "g1", [128, 8])
    swin = din(nc, "swin", [D, 3 * D])
    scw = din(nc, "scw", [128, 8, 3])
    swout = din(nc, "swout", [D, D])
    f1 = ffn_inputs(nc, "f1")
    g2 = din(nc, "g2", [128, 8])
    gwin = din(nc, "gwin", [D, 4 * D + 16])
    gcw = din(nc, "gcw", [128, 24, 4])
    xm0 = dtmp(nc, "xm0", [D, T])
    x1 = dtmp(nc, "x1", [D, T])
    xm1 = dtmp(nc, "xm1", [D, T])
    x2 = dout(nc, "x2T", [D, T])
    gp = dout(nc, "gpT", [4 * D + 16, T])
    with ExitStack() as es:
        p = Prog(nc, es)
        pass_oproj(p, T, xT, oT, BF16, wo, xm0)
        pass_ffn(p, T, xm0, f0["g"], f0["wup"], f0["cw"], f0["wdn"], x1)
        pass_sconv(p, T, x1, g1, swin, scw, swout, xm1)
        pass_ffn(p, T, xm1, f1["g"], f1["wup"], f1["cw"], f1["wdn"], x2)
        pass_proj(p, T, x2, g2, gwin, 4 * D + 16, gp, F32, nconv=24, K=4, cw_ap=gcw)
        p.finish()
    return nc


def build_E(T):
    nc = new_nc()
    xT = din(nc, "xT", [D, T])
    yT = din(nc, "yT", [D, T])
    wo = din(nc, "wo", [D, D])
    f2 = ffn_inputs(nc, "f2")
    g3 = din(nc, "g3", [128, 8])
    w3 = din(nc, "w3", [D, 3 * D])
    xm = dtmp(nc, "xm", [D, T])
    x3 = dout(nc, "x3T", [D, T])
    qkv = dout(nc, "qkvT", [3 * D, T], BF16)
    with ExitStack() as es:
        p = Prog(nc, es)
        pass_oproj(p, T, xT, yT, F32, wo, xm)
        pass_ffn(p, T, xm, f2["g"], f2["wup"], f2["cw"], f2["wdn"], x3)
        pass_proj(p, T, x3, g3, w3, 3 * D, qkv, BF16, scale_chunks=range(8), scale=HD ** -0.5)
        p.finish()
    return nc


def gdn_consts():
    i = np.arange(64)
    U = (i[:, None] <= i[None, :]).astype(np.float32)
    SL = (i[:, None] > i[None, :]).astype(np.float32)
    SU = (i[None, :] > i[:, None]).astype(np.float32)
    UI = (i[None, :] >= i[:, None]).astype(np.float32)
    return dict(U=U, SL=SL, SU=SU, UI=UI, id64=np.eye(64, dtype=np.float32))


def moba_attn(qkv, S):
    ncB = cached(("B", S), lambda: build_B(S))
    maps = []
    for h in range(NH):
        m = dict(qT=np.ascontiguousarray(qkv[:, :, h * 128:(h + 1) * 128].transpose(0, 2, 1)),
                 kT=np.ascontiguousarray(qkv[:, :, D + h * 128:D + (h + 1) * 128].transpose(0, 2, 1)),
                 v=np.ascontiguousarray(qkv[:, :, 2 * D + h * 128:2 * D + (h + 1) * 128]))
        m.update(moba_consts(h))
        maps.append(m)
    resB = run(ncB, maps)
    return np.concatenate([r["oT"].transpose(0, 2, 1) for r in resB], axis=2)


def forward(inp, S, dbg=None):
    f32 = np.float32
    inp = {k_: np.asarray(v_, dtype=f32) for k_, v_ in inp.items()}
    T = HALO + S // 4
    x = inp["x"]

    def fm(tag, i):
        return ffn_maps(tag, inp["ffn_norm"][i], inp["ffn_w_up"][i], inp["ffn_conv"][i], inp["ffn_w_down"][i])

    ncA = cached(("A", T), lambda: build_A(T))
    xs = shard_T(x, S)
    resA = run(ncA, [{"xT": xs[c], "g": vec128(inp["mix_norm"][0]), "w": inp["moba_w_qkv"][0]} for c in range(NCORE)])
    qkv = unshard_T([r["qkvT"] for r in resA], S)
    os_ = shard_T(moba_attn(qkv, S), S)
    ncC = cached(("C", T), lambda: build_C(T))
    maps = []
    for c in range(NCORE):
        m = {"xT": xs[c], "oT": os_[c], "wo": inp["moba_w_o"][0], "g1": vec128(inp["mix_norm"][1]),
             "swin": inp["sconv_w_in"][0], "scw": conv128(inp["sconv_conv"][0]), "swout": inp["sconv_w_out"][0],
             "g2": vec128(inp["mix_norm"][2]), "gwin": inp["gdn_w_in"][0], "gcw": conv128(inp["gdn_conv"][0])}
        m.update(fm("f0", 0))
        m.update(fm("f1", 1))
        maps.append(m)
    resC = run(ncC, maps)
    x2 = unshard_T([r["x2T"] for r in resC], S)
    gp = unshard_T([r["gpT"] for r in resC], S)
    if dbg is not None:
        dbg["ffn1"] = x2
        dbg["gp"] = gp
    ncD = cached(("D", S), lambda: build_D(S))
    NCH = S // 64
    gc_ = gdn_consts()
    maps = []
    for h in range(NH):
        m = dict(q=np.ascontiguousarray(gp[:, :, h * 128:(h + 1) * 128]),
                 k=np.ascontiguousarray(gp[:, :, D + h * 128:D + (h + 1) * 128]),
                 v=np.ascontiguousarray(gp[:, :, 2 * D + h * 128:2 * D + (h + 1) * 128]),
                 z=np.ascontiguousarray(gp[:, :, 3 * D + h * 128:3 * D + (h + 1) * 128]),
                 btab=np.ascontiguousarray(gp[:, :, 4 * D + h].reshape(2, NCH, 64).transpose(0, 2, 1)),
                 atab=np.ascontiguousarray(gp[:, :, 4 * D + NH + h].reshape(2, NCH, 64).transpose(0, 2, 1)),
                 alog=np.full((128, 1), inp["gdn_a_log"][0][h], f32),
                 dtb=np.full((128, 1), inp["gdn_dt_bias"][0][h], f32),
                 normw=np.ascontiguousarray(np.broadcast_to(inp["gdn_norm"][0][None, :], (64, 128))))
        m.update(gc_)
        maps.append(m)
    resD = run(ncD, maps)
    yfull = np.concatenate([r["y"] for r in resD], axis=2)
    if dbg is not None:
        dbg["y"] = yfull
    ncE = cached(("E", T), lambda: build_E(T))
    x2s = shard_T(x2, S)
    ys = shard_T(yfull, S)
    maps = []
    for c in range(NCORE):
        m = {"xT": x2s[c], "yT": ys[c], "wo": inp["gdn_w_o"][0], "g3": vec128(inp["mix_norm"][3]),
             "w3": inp["moba_w_qkv"][1]}
        m.update(fm("f2", 2))
        maps.append(m)
    resE = run(ncE, maps)
    x3 = unshard_T([r["x3T"] for r in resE], S)
    qkv = unshard_T([r["qkvT"] for r in resE], S)
    if dbg is not None:
        dbg["ffn2"] = x3
    os_ = shard_T(moba_attn(qkv, S), S)
    ncG = cached(("G", T), lambda: build_G(T, True))
    x3s = shard_T(x3, S)
    maps = []
    for c in range(NCORE):
        m = {"xT": x3s[c], "oT": os_[c], "wo": inp["moba_w_o"][1], "gf": vec128(inp["final_norm"])}
        m.update(fm("f", 3))
        maps.append(m)
    resG = run(ncG, maps)
    return unshard_T([r["xoT"] for r in resG], S)


def kernel(**inputs):
    S = inputs["x"].shape[1]
    return forward(inputs, S).astype(np.float32)
```
